# Optimizing a Trainium2 kernel written in Bass

```python
import jax, jax.numpy as jnp
from jax import lax
import numpy as np

D_MODEL = 1024
BATCH = 16
SEQ = 2048
DEPTH = 4

CTX_LEN = 256
GRID_W = 64
HEAD_DIM = 64
N_Q_HEADS = 12
N_KV_HEADS = 4
Q_BLOCK = 128
ROPE_THETA = 10000.0
AXIS_DIM = HEAD_DIM // 2
N_FOURIER_GROUPS = 4
FOURIER_GROUP_DIM = 64
CHUNK = 128
N_SGU_GROUPS = 4
SGU_GROUP_DIM = 128
CONV_WIDTH = 31
N_CONV_GROUPS = 4
CONV_GROUP_DIM = 128
N_EXPERT_GROUPS = 4
EXPERTS_PER_GROUP = 8
N_EXPERTS = N_EXPERT_GROUPS * EXPERTS_PER_GROUP
TOP_K = 2
D_EXPERT = 256

Q_W = N_Q_HEADS * HEAD_DIM
KV_W = N_KV_HEADS * HEAD_DIM
F_W = N_FOURIER_GROUPS * FOURIER_GROUP_DIM
AB_IN = Q_W + 2 * KV_W + F_W
AB_OUT = Q_W + F_W
C_W = N_SGU_GROUPS * SGU_GROUP_DIM
D_W = N_CONV_GROUPS * CONV_GROUP_DIM
CD_IN = 2 * C_W + 2 * D_W
CD_OUT = C_W + D_W
N_EVEN = (DEPTH + 1) // 2
N_ODD = DEPTH // 2
ALPHA = (2 * DEPTH) ** 0.25
BETA = (8 * DEPTH) ** -0.25
EPS = 1e-6

kernel_name = "hybrid_dit_attn_fourier_sgu_conv_hmoe"


def layer_norm(x, g, b):
    xf = x.astype(jnp.float32)
    mu = jnp.mean(xf, axis=-1, keepdims=True)
    var = jnp.mean(jnp.square(xf - mu), axis=-1, keepdims=True)
    return ((xf - mu) * lax.rsqrt(var + EPS) * g + b).astype(x.dtype)


def rms_norm(x, g):
    xf = x.astype(jnp.float32)
    return (xf * lax.rsqrt(jnp.mean(jnp.square(xf), axis=-1, keepdims=True) + EPS) * g).astype(x.dtype)


def rope_tables(n):
    rows = n // GRID_W
    row = jnp.repeat(jnp.arange(rows, dtype=jnp.float32), GRID_W)
    col = jnp.tile(jnp.arange(GRID_W, dtype=jnp.float32), rows)
    inv_freq = 1.0 / (ROPE_THETA ** (jnp.arange(0, AXIS_DIM, 2, dtype=jnp.float32) / AXIS_DIM))
    ang = jnp.concatenate([row[:, None] * inv_freq, col[:, None] * inv_freq], axis=-1)
    return jnp.cos(ang), jnp.sin(ang)


def apply_rope(x, cos, sin):
    xr = x.astype(jnp.float32).reshape(x.shape[:-1] + (HEAD_DIM // 2, 2))
    x0, x1 = xr[..., 0], xr[..., 1]
    cs, sn = cos[None, :, None, :], sin[None, :, None, :]
    out = jnp.stack([x0 * cs - x1 * sn, x0 * sn + x1 * cs], axis=-1)
    return out.reshape(x.shape).astype(x.dtype)


def gqa_core(q, k, v):
    s = jnp.einsum('bqgrd,bkgd->bgrqk', q, k, preferred_element_type=jnp.float32) * (HEAD_DIM ** -0.5)
    p = jax.nn.softmax(s, axis=-1).astype(v.dtype)
    return jnp.einsum('bgrqk,bkgd->bqgrd', p, v)


def block_attention(q, k, v):
    bsz, n = q.shape[:2]
    rep = N_Q_HEADS // N_KV_HEADS
    qb = q.reshape(bsz, n // Q_BLOCK, Q_BLOCK, N_KV_HEADS, rep, HEAD_DIM).transpose(1, 0, 2, 3, 4, 5)
    out = lax.map(lambda blk: gqa_core(blk, k, v), qb)
    return out.transpose(1, 0, 2, 3, 4, 5).reshape(bsz, n, Q_W)


def fourier_mix(f, w, b):
    bsz, n, _ = f.shape
    fg = f.reshape(bsz, n, N_FOURIER_GROUPS, FOURIER_GROUP_DIM).astype(jnp.float32)
    mixed = jnp.fft.fft2(fg, axes=(1, 3), norm='ortho').real.astype(f.dtype)
    return (jnp.einsum('bngc,gcd->bngd', mixed, w) + b).reshape(bsz, n, F_W)


def mixer_ab(h_lat, h_ctx, ctx_out, w_in, w_out, q_gain, k_gain, w_f, b_f):
    bsz, n, _ = h_lat.shape
    lc = h_ctx.shape[1]
    rep = N_Q_HEADS // N_KV_HEADS
    q, k, v, f = jnp.split(h_lat @ w_in, [Q_W, Q_W + KV_W, Q_W + 2 * KV_W], axis=-1)
    cos, sin = rope_tables(n)
    q = apply_rope(rms_norm(q.reshape(bsz, n, N_Q_HEADS, HEAD_DIM), q_gain), cos, sin)
    k = apply_rope(rms_norm(k.reshape(bsz, n, N_KV_HEADS, HEAD_DIM), k_gain), cos, sin)
    v = v.reshape(bsz, n, N_KV_HEADS, HEAD_DIM)
    if ctx_out:
        qc, kc, vc, fc = jnp.split(h_ctx @ w_in, [Q_W, Q_W + KV_W, Q_W + 2 * KV_W], axis=-1)
    else:
        kc, vc = jnp.split(h_ctx @ w_in[:, Q_W:Q_W + 2 * KV_W], [KV_W], axis=-1)
    kc = rms_norm(kc.reshape(bsz, lc, N_KV_HEADS, HEAD_DIM), k_gain)
    vc = vc.reshape(bsz, lc, N_KV_HEADS, HEAD_DIM)
    k_all = jnp.concatenate([kc, k], axis=1)
    v_all = jnp.concatenate([vc, v], axis=1)
    a_lat = block_attention(q, k_all, v_all)
    y_lat = jnp.concatenate([a_lat, fourier_mix(f, w_f, b_f)], axis=-1) @ w_out
    if not ctx_out:
        return y_lat, None
    qc = rms_norm(qc.reshape(bsz, lc, N_Q_HEADS, HEAD_DIM), q_gain).reshape(bsz, lc, N_KV_HEADS, rep, HEAD_DIM)
    a_ctx = gqa_core(qc, kc, vc).reshape(bsz, lc, Q_W)
    y_ctx = jnp.concatenate([a_ctx, fourier_mix(fc, w_f, b_f)], axis=-1) @ w_out
    return y_lat, y_ctx


def mixer_cd(h, w_in, w_out, sgu_g, sgu_b, w_sp, b_sp, conv_w, conv_b, cn_g, cn_b):
    bsz, n, _ = h.shape
    p = h @ w_in
    u, v = jnp.split(jax.nn.gelu(p[..., :2 * C_W]), 2, axis=-1)
    a, gate = jnp.split(p[..., 2 * C_W:], 2, axis=-1)
    vg = layer_norm(v.reshape(bsz, n // CHUNK, CHUNK, N_SGU_GROUPS, SGU_GROUP_DIM), sgu_g, sgu_b)
    sv = jnp.einsum('gpq,bnqgc->bnpgc', w_sp, vg) + b_sp.T[:, :, None]
    y_c = u * sv.reshape(bsz, n, C_W)
    glu = a * jax.nn.sigmoid(gate)
    dw = lax.conv_general_dilated(glu, conv_w[:, None, :], window_strides=(1,),
                                  padding=[(CONV_WIDTH // 2, CONV_WIDTH // 2)],
                                  dimension_numbers=('NWC', 'WIO', 'NWC'),
                                  feature_group_count=D_W) + conv_b
    dn = layer_norm(dw.reshape(bsz, n, N_CONV_GROUPS, CONV_GROUP_DIM), cn_g, cn_b).reshape(bsz, n, D_W)
    y_d = jax.nn.silu(dn)
    return jnp.concatenate([y_c, y_d], axis=-1) @ w_out


def hier_moe(t, w_group, b_group, w_router, b_router, w_gate, w_up, w_down):
    g_logits = (t @ w_group + b_group).astype(jnp.float32)
    g_prob = jax.nn.softmax(g_logits, axis=-1)
    g_idx = jnp.argmax(g_logits, axis=-1)
    g_w = jnp.take_along_axis(g_prob, g_idx[:, None], axis=-1)
    e_logits = (jnp.einsum('td,gde->tge', t, w_router) + b_router).astype(jnp.float32)
    e_sel = jnp.take_along_axis(e_logits, g_idx[:, None, None], axis=1)[:, 0]
    top_v, top_i = lax.top_k(e_sel, TOP_K)
    top_w = jax.nn.softmax(top_v, axis=-1) * g_w
    expert_id = g_idx[:, None] * EXPERTS_PER_GROUP + top_i
    combine = jnp.einsum('tk,tke->te', top_w,
                         jax.nn.one_hot(expert_id, N_EXPERTS, dtype=jnp.float32)).astype(t.dtype)
    out = jnp.zeros_like(t)
    for e in range(N_EXPERTS):
        hid = jax.nn.silu(t @ w_gate[e]) * (t @ w_up[e])
        out = out + combine[:, e:e + 1] * (hid @ w_down[e])
    return out


def setup_inputs(seed: int = 0) -> dict:
    key = jax.random.key(seed)
    ks = iter(jax.random.split(key, 40))
    f32 = jnp.float32

    def nrm(shape, scale):
        return jax.random.normal(next(ks), shape, f32) * scale

    def gain(shape):
        return 1.0 + nrm(shape, 0.05)

    return {
        "x": nrm((BATCH, SEQ, D_MODEL), 1.0),
        "c": nrm((BATCH, D_MODEL), 1.0),
        "ctx": nrm((BATCH, CTX_LEN, D_MODEL), 1.0),
        "c_ctx": nrm((D_MODEL,), 1.0),
        "w_mod": nrm((DEPTH, D_MODEL, 6 * D_MODEL), 0.5 * D_MODEL ** -0.5),
        "b_mod": nrm((DEPTH, 6 * D_MODEL), 0.02),
        "ln_g": gain((DEPTH, 2, D_MODEL)),
        "ln_b": nrm((DEPTH, 2, D_MODEL), 0.02),
        "w_in_ab": nrm((N_EVEN, D_MODEL, AB_IN), D_MODEL ** -0.5),
        "w_out_ab": nrm((N_EVEN, AB_OUT, D_MODEL), BETA * AB_OUT ** -0.5),
        "q_gain": gain((N_EVEN, HEAD_DIM)),
        "k_gain": gain((N_EVEN, HEAD_DIM)),
        "w_fourier": nrm((N_EVEN, N_FOURIER_GROUPS, FOURIER_GROUP_DIM, FOURIER_GROUP_DIM), FOURIER_GROUP_DIM ** -0.5),
        "b_fourier": nrm((N_EVEN, N_FOURIER_GROUPS, FOURIER_GROUP_DIM), 0.02),
        "w_in_cd": nrm((N_ODD, D_MODEL, CD_IN), D_MODEL ** -0.5),
        "w_out_cd": nrm((N_ODD, CD_OUT, D_MODEL), BETA * CD_OUT ** -0.5),
        "sgu_g": gain((N_ODD, N_SGU_GROUPS, SGU_GROUP_DIM)),
        "sgu_b": nrm((N_ODD, N_SGU_GROUPS, SGU_GROUP_DIM), 0.02),
        "w_spatial": nrm((N_ODD, N_SGU_GROUPS, CHUNK, CHUNK), CHUNK ** -0.5),
        "b_spatial": gain((N_ODD, N_SGU_GROUPS, CHUNK)),
        "conv_w": nrm((N_ODD, CONV_WIDTH, D_W), CONV_WIDTH ** -0.5),
        "conv_b": nrm((N_ODD, D_W), 0.02),
        "conv_norm_g": gain((N_ODD, N_CONV_GROUPS, CONV_GROUP_DIM)),
        "conv_norm_b": nrm((N_ODD, N_CONV_GROUPS, CONV_GROUP_DIM), 0.02),
        "w_group": nrm((DEPTH, D_MODEL, N_EXPERT_GROUPS), D_MODEL ** -0.5),
        "b_group": nrm((DEPTH, N_EXPERT_GROUPS), 0.01),
        "w_router": nrm((DEPTH, N_EXPERT_GROUPS, D_MODEL, EXPERTS_PER_GROUP), D_MODEL ** -0.5),
        "b_router": nrm((DEPTH, N_EXPERT_GROUPS, EXPERTS_PER_GROUP), 0.01),
        "w_exp_gate": nrm((DEPTH, N_EXPERTS, D_MODEL, D_EXPERT), D_MODEL ** -0.5),
        "w_exp_up": nrm((DEPTH, N_EXPERTS, D_MODEL, D_EXPERT), D_MODEL ** -0.5),
        "w_exp_down": nrm((DEPTH, N_EXPERTS, D_EXPERT, D_MODEL), BETA * D_EXPERT ** -0.5),
    }


def reference(x, c, ctx, c_ctx, w_mod, b_mod, ln_g, ln_b, w_in_ab, w_out_ab, q_gain, k_gain,
              w_fourier, b_fourier, w_in_cd, w_out_cd, sgu_g, sgu_b, w_spatial, b_spatial,
              conv_w, conv_b, conv_norm_g, conv_norm_b, w_group, b_group, w_router, b_router,
              w_exp_gate, w_exp_up, w_exp_down):
    x_lat, x_ctx = x, ctx
    s_c = jax.nn.silu(c)
    s_cc = jax.nn.silu(c_ctx)
    for l in range(DEPTH):
        ctx_out = any(j % 2 == 0 for j in range(l + 1, DEPTH))
        i = l // 2
        sh1, sc1, g1, sh2, sc2, g2 = jnp.split((s_c @ w_mod[l] + b_mod[l])[:, None, :], 6, axis=-1)
        sh1c, sc1c, g1c, sh2c, sc2c, g2c = jnp.split(s_cc @ w_mod[l] + b_mod[l], 6, axis=-1)
        h_lat = x_lat * (1.0 + sc1) + sh1
        if l % 2 == 0:
            h_ctx = x_ctx * (1.0 + sc1c) + sh1c
            y_lat, y_ctx = mixer_ab(h_lat, h_ctx, ctx_out, w_in_ab[i], w_out_ab[i], q_gain[i], k_gain[i],
                                    w_fourier[i], b_fourier[i])
        else:
            cd_params = (w_in_cd[i], w_out_cd[i], sgu_g[i], sgu_b[i], w_spatial[i], b_spatial[i],
                         conv_w[i], conv_b[i], conv_norm_g[i], conv_norm_b[i])
            y_lat = mixer_cd(h_lat, *cd_params)
            if ctx_out:
                y_ctx = mixer_cd(x_ctx * (1.0 + sc1c) + sh1c, *cd_params)
        x_lat = layer_norm(ALPHA * x_lat + g1 * y_lat, ln_g[l, 0], ln_b[l, 0])
        moe_params = (w_group[l], b_group[l], w_router[l], b_router[l], w_exp_gate[l], w_exp_up[l], w_exp_down[l])
        h_lat = (x_lat * (1.0 + sc2) + sh2).reshape(-1, D_MODEL)
        n_lat = h_lat.shape[0]
        if ctx_out:
            x_ctx = layer_norm(ALPHA * x_ctx + g1c * y_ctx, ln_g[l, 0], ln_b[l, 0])
            h_ctx = (x_ctx * (1.0 + sc2c) + sh2c).reshape(-1, D_MODEL)
            y_all = hier_moe(jnp.concatenate([h_lat, h_ctx], axis=0), *moe_params)
            y_lat = y_all[:n_lat].reshape(x_lat.shape)
            x_ctx = layer_norm(ALPHA * x_ctx + g2c * y_all[n_lat:].reshape(x_ctx.shape), ln_g[l, 1], ln_b[l, 1])
        else:
            y_lat = hier_moe(h_lat, *moe_params).reshape(x_lat.shape)
            x_ctx = None
        x_lat = layer_norm(ALPHA * x_lat + g2 * y_lat, ln_g[l, 1], ln_b[l, 1])
    return x_lat
```

```python
import numpy as np
import concourse.bass as bass
import concourse.mybir as mybir

F32 = mybir.dt.float32
BF16 = mybir.dt.bfloat16
AF = mybir.ActivationFunctionType
ALU = mybir.AluOpType
AX = mybir.AxisListType

STRICT_ALL = True
SB_PAGE = 256
PS_PAGE = 2048
ARENA0 = 16640
ARENA1 = 229376 - 64
ESZ = {F32: 4, BF16: 2}


class Op:
    __slots__ = ("eng", "fn", "deps", "dma", "sig", "sem", "target", "idx", "extra_wait", "strict")

    def __init__(self, eng, fn, dma):
        self.eng = eng
        self.fn = fn
        self.dma = dma
        self.deps = set()
        self.sig = False
        self.sem = None
        self.target = 0
        self.idx = 0
        self.extra_wait = None


class Sched:
    ENGS = ("pe", "act", "dve", "pool", "sp")

    def __init__(self, nc):
        self.nc = nc
        self.ops = []
        self.base = {}
        self.lastw = {}
        self.readers = {}
        self.dma_count = {"sp": 0, "pool": 0, "act": 0}
        self.dma_hist = {"sp": [], "pool": [], "act": []}
        self.POOLSZ = {"sp": 16, "pool": 16, "act": 8}
        self.npsum = 0
        self.strict = False

    def sb(self, name, shape, dtype, offset):
        assert offset >= ARENA0 and offset % 32 == 0, (name, offset)
        nbytes = int(np.prod(shape[1:])) * ESZ[dtype]
        assert offset + nbytes <= ARENA1, (name, offset, nbytes)
        h = self.nc.alloc_sbuf_tensor_at(name, list(shape), dtype, offset=offset)
        self.base[h.name] = ("sb", offset)
        return h

    def ps(self, name, shape=(128, 512), dtype=F32):
        h = self.nc.alloc_psum_tensor(name, list(shape), dtype)
        self.base[h.name] = ("ps", self.npsum * 2048)
        self.npsum += 1
        return h

    def pages(self, ap):
        tn = ap.tensor.name
        if tn not in self.base:
            return ()
        space, base = self.base[tn]
        esz = ESZ.get(ap.dtype, None)
        if esz is None:
            esz = ap.nbytes() // max(1, ap.size())
        pat = ap.ap
        pstep, pn = pat[0]
        off = int(ap.offset)
        p0 = off // pstep if pstep > 0 else 0
        foff = off - p0 * pstep
        ext = 0
        for st, cnt in pat[1:]:
            ext += (cnt - 1) * abs(st)
        b0 = base + foff * esz
        b1 = base + (foff + ext + 1) * esz
        pg = SB_PAGE if space == "sb" else PS_PAGE
        q0 = p0 // 32
        q1 = (p0 + pn - 1) // 32
        out = []
        for pgi in range(b0 // pg, (b1 - 1) // pg + 1):
            for q in range(q0, q1 + 1):
                out.append((space, pgi, q))
        return out

    def op(self, eng, fn, r=(), w=(), dma=False):
        o = Op(eng, fn, dma)
        o.strict = self.strict
        i = len(self.ops)
        o.idx = i
        deps = o.deps
        r = list(r)
        w = list(w)
        for ap in list(r):
            if ap is None or isinstance(ap, (int, float)):
                continue
            if self.base.get(ap.tensor.name, ("", 0))[0] == "ps":
                w.append(ap)
                continue
            for k in self.pages(ap):
                lw = self.lastw.get(k)
                if lw is not None:
                    deps.add(lw)
                self.readers.setdefault(k, []).append(i)
        for ap in w:
            for k in self.pages(ap):
                lw = self.lastw.get(k)
                if lw is not None:
                    deps.add(lw)
                rs = self.readers.get(k)
                if rs:
                    deps.update(rs)
                    self.readers[k] = []
                self.lastw[k] = i
        deps.discard(i)
        if dma:
            q = eng
            n = self.dma_count[q]
            self.dma_count[q] = n + 1
            hist = self.dma_hist[q]
            P = self.POOLSZ[q]
            o.sem = (q, n % P)
            o.target = 16 * (n // P + 1)
            if n >= P:
                o.extra_wait = hist[n - P]
            hist.append(i)
        self.ops.append(o)
        return o

    def emit(self):
        nc = self.nc
        ops = self.ops
        for o in ops:
            keep = set()
            for d in o.deps:
                p = ops[d]
                if (not p.dma) and (not o.dma) and p.eng == o.eng and (o.eng == "pe" or not (o.strict or STRICT_ALL)):
                    continue
                keep.add(d)
            if o.extra_wait is not None:
                keep.add(o.extra_wait)
            o.deps = keep
            for d in keep:
                if not ops[d].dma:
                    ops[d].sig = True
        cnt = {e: 0 for e in self.ENGS}
        sigidx = {}
        for o in ops:
            if o.sig and not o.dma:
                cnt[o.eng] += 1
                sigidx[o.idx] = cnt[o.eng]
        from contextlib import ExitStack
        with ExitStack() as es:
            esem = {e: es.enter_context(nc.semaphore("s_" + e)) for e in self.ENGS}
            dsem = {}
            for q, P in self.POOLSZ.items():
                for j in range(P):
                    dsem[(q, j)] = es.enter_context(nc.semaphore(f"d_{q}{j}"))
            block = es.enter_context(nc.Block())
            per = {e: [o for o in ops if o.eng == e] for e in self.ENGS}
            final = {}
            for o in ops:
                if o.dma:
                    final[o.sem] = max(final.get(o.sem, 0), o.target)

            def run(ename, eng):
                waited = {}
                for o in per[ename]:
                    need = {}
                    for d in o.deps:
                        p = ops[d]
                        if p.dma:
                            s = dsem[p.sem]
                            v = p.target
                        else:
                            s = esem[p.eng]
                            v = sigidx[p.idx]
                        key = id(s)
                        if need.get(key, (None, 0))[1] < v:
                            need[key] = (s, v)
                    for key, (s, v) in need.items():
                        if waited.get(key, 0) >= v:
                            continue
                        eng.wait_ge(s, v)
                        waited[key] = v
                    ins = o.fn(eng)
                    if o.dma:
                        ins.then_inc(dsem[o.sem], 16)
                    elif o.sig:
                        ins.then_inc(esem[ename], 1)
                if ename == "sp":
                    for k, v in final.items():
                        eng.wait_ge(dsem[k], v)

            @block.tensor
            def _(e):
                run("pe", e)

            @block.scalar
            def _(e):
                run("act", e)

            @block.vector
            def _(e):
                run("dve", e)

            @block.gpsimd
            def _(e):
                run("pool", e)

            @block.sync
            def _(e):
                run("sp", e)

    def mm(self, out, lhsT, rhs, start=True, stop=True):
        return self.op("pe", lambda e: e.matmul(out, lhsT, rhs, start=start, stop=stop), r=(lhsT, rhs), w=(out,))

    def transpose(self, out, in_, ident):
        return self.op("pe", lambda e: e.transpose(out, in_, ident), r=(in_, ident), w=(out,))

    def act(self, out, in_, func, bias=0.0, scale=1.0, accum_out=None, eng="act"):
        r = [in_]
        if not isinstance(bias, (int, float)):
            r.append(bias)
        if not isinstance(scale, (int, float)):
            r.append(scale)
        w = [out]
        kw = {}
        if accum_out is not None:
            w.append(accum_out)
            kw["accum_out"] = accum_out
        return self.op("act", lambda e: e.activation(out, in_, func, bias=bias, scale=scale, **kw), r=r, w=w)

    def tt(self, out, in0, in1, op, eng="dve"):
        return self.op(eng, lambda e: e.tensor_tensor(out, in0, in1, op), r=(in0, in1), w=(out,))

    def ts(self, out, in0, s1, s2=None, op0=ALU.mult, op1=None, eng="dve"):
        r = [in0]
        if not isinstance(s1, (int, float)):
            r.append(s1)
        if s2 is not None and not isinstance(s2, (int, float)):
            r.append(s2)
        if op1 is None:
            return self.op(eng, lambda e: e.tensor_scalar(out, in0, s1, None, op0), r=r, w=(out,))
        return self.op(eng, lambda e: e.tensor_scalar(out, in0, s1, s2, op0, op1), r=r, w=(out,))

    def stt(self, out, in0, scalar, in1, op0, op1):
        r = [in0, in1]
        if not isinstance(scalar, (int, float)):
            r.append(scalar)
        return self.op("dve", lambda e: e.scalar_tensor_tensor(out, in0, scalar, in1, op0, op1), r=r, w=(out,))

    def copy(self, out, in_, eng="dve"):
        if eng == "act":
            return self.op("act", lambda e: e.copy(out, in_), r=(in_,), w=(out,))
        return self.op(eng, lambda e: e.tensor_copy(out, in_), r=(in_,), w=(out,))

    def recip(self, out, in_):
        return self.op("dve", lambda e: e.reciprocal(out, in_), r=(in_,), w=(out,))

    def memset(self, out, val, eng="dve"):
        return self.op(eng, lambda e: e.memset(out, val), r=(), w=(out,))

    def dma(self, q, out, in_, **kw):
        return self.op(q, lambda e: e.dma_start(out=out, in_=in_, **kw), r=(in_,), w=(out,), dma=True)
import os
import numpy as np
import ml_dtypes
from concourse.bass_utils import run_bass_kernel_spmd

D = 1024
T = 2304
NCTX = 256
NLAT = 2048
DEPTH = 4
ALPHA = (2 * DEPTH) ** 0.25
EPS = 1e-6
TILES = [(0, 256), (256, 512), (768, 512), (1280, 512), (1792, 512)]
QCH = [(0, 3), (1, 4), (2, 5), (6, 9), (7, 10), (8, 11)]
NWIN = 2560


def host_prep(inp):
    f32 = np.float32
    sh = {}
    sh["w_mod"] = np.ascontiguousarray(inp["w_mod"], dtype=f32)
    sh["b_mod"] = np.ascontiguousarray(inp["b_mod"].reshape(4, 48, 128).transpose(2, 0, 1).reshape(128, 4 * 48))
    sh["ln_g"] = np.ascontiguousarray(inp["ln_g"].reshape(4, 2, 8, 128).transpose(3, 0, 1, 2).reshape(128, 64))
    sh["ln_b"] = np.ascontiguousarray(inp["ln_b"].reshape(4, 2, 8, 128).transpose(3, 0, 1, 2).reshape(128, 64))
    partner = np.arange(64) ^ 1
    cols = []
    def head_cols(base, h, part):
        idx = base + h * 64 + (partner if part else np.arange(64))
        return idx
    for (ha, hb) in QCH:
        cols.append(np.concatenate([head_cols(0, ha, False), head_cols(0, hb, False)]))
        cols.append(np.concatenate([head_cols(0, ha, True), head_cols(0, hb, True)]))
    for (ga, gb) in [(0, 1), (2, 3)]:
        cols.append(np.concatenate([head_cols(768, ga, False), head_cols(768, gb, False)]))
        cols.append(np.concatenate([head_cols(768, ga, True), head_cols(768, gb, True)]))
    cols.append(np.arange(1280, 1536))
    cols.append(np.arange(1024, 1280))
    cols = np.concatenate(cols)
    assert cols.shape[0] == NWIN
    sh["w_in_ab"] = np.ascontiguousarray(inp["w_in_ab"][:, :, cols])
    rows = np.concatenate([np.concatenate([np.arange(ha * 64, ha * 64 + 64), np.arange(hb * 64, hb * 64 + 64)]) for ha, hb in QCH] + [np.arange(768, 1024)])
    sh["w_out_ab"] = np.ascontiguousarray(inp["w_out_ab"][:, rows, :])
    g = np.zeros((128, 2, 4), f32)
    for i in range(2):
        g[:, i, 0] = np.tile(inp["q_gain"][i], 2)
        g[:, i, 1] = np.tile(inp["q_gain"][i][partner], 2)
        g[:, i, 2] = np.tile(inp["k_gain"][i], 2)
        g[:, i, 3] = np.tile(inp["k_gain"][i][partner], 2)
    sh["gains"] = g.reshape(128, 8)
    rows_ = NLAT // 64
    row = np.repeat(np.arange(rows_, dtype=f32), 64)
    col = np.tile(np.arange(64, dtype=f32), rows_)
    inv_freq = (1.0 / (10000.0 ** (np.arange(0, 32, 2, dtype=f32) / 32))).astype(f32)
    ang = np.concatenate([row[:, None] * inv_freq, col[:, None] * inv_freq], axis=-1).astype(f32)
    cos = np.cos(ang).astype(f32)
    sin = np.sin(ang).astype(f32)
    d = np.arange(128) % 64
    cosT = cos[:, d // 2].T
    sgn = np.where(d % 2 == 0, -1.0, 1.0).astype(f32)
    sinT = sin[:, d // 2].T * sgn[:, None]
    sh["rope"] = np.ascontiguousarray(np.stack([cosT, sinT], axis=1).astype(f32))
    def dft(n, scale):
        k = np.arange(n, dtype=np.int64)
        m = (k[:, None] * k[None, :]) % n
        a = 2.0 * np.pi * m.astype(np.float64) / n
        return (np.cos(a) * scale), (np.sin(a) * scale)
    c2048, s2048 = dft(2048, 1.0 / np.sqrt(2048.0))
    sh["dftc"] = c2048.astype(ml_dtypes.bfloat16)
    sh["dfts"] = s2048.astype(ml_dtypes.bfloat16)
    c256, s256 = dft(256, 1.0 / 16.0)
    sh["dftc256"] = c256.astype(ml_dtypes.bfloat16)
    sh["dfts256"] = s256.astype(ml_dtypes.bfloat16)
    c64, s64 = dft(64, 1.0 / 8.0)
    bd = np.zeros((128, 2, 128), f32)
    bd[0:64, 0, 0:64] = c64
    bd[64:128, 0, 64:128] = c64
    bd[0:64, 1, 0:64] = -s64
    bd[64:128, 1, 64:128] = -s64
    sh["dft64"] = bd.reshape(128, 256).astype(ml_dtypes.bfloat16)
    wf = np.zeros((2, 128, 2, 128), f32)
    for i in range(2):
        for gg in range(4):
            ch, o = gg // 2, (gg % 2) * 64
            wf[i, o:o + 64, ch, o:o + 64] = inp["w_fourier"][i, gg]
    sh["w_fourier"] = wf.reshape(2, 128, 256)
    sh["b_fourier"] = np.ascontiguousarray(inp["b_fourier"].reshape(2, 2, 128).transpose(2, 0, 1).reshape(128, 4))
    ccols = [np.arange(0, 1024)]
    for c in range(4):
        ccols.append(np.arange(1024 + c * 128, 1024 + (c + 1) * 128))
        ccols.append(np.arange(1536 + c * 128, 1536 + (c + 1) * 128))
    sh["w_in_cd"] = np.ascontiguousarray(inp["w_in_cd"][:, :, np.concatenate(ccols)])
    sh["w_out_cd"] = np.ascontiguousarray(inp["w_out_cd"], dtype=f32)
    sh["sgu_gb"] = np.ascontiguousarray(np.stack([inp["sgu_g"].reshape(2, 512), inp["sgu_b"].reshape(2, 512)], axis=1))
    sh["w_spT"] = np.ascontiguousarray(inp["w_spatial"].transpose(0, 3, 1, 2).reshape(2, 128, 512))
    sh["b_sp"] = np.ascontiguousarray(inp["b_spatial"].reshape(2, 512))
    sh["conv_w"] = np.ascontiguousarray(inp["conv_w"].reshape(2, 31, 4, 128).transpose(3, 0, 2, 1).reshape(128, 2 * 4 * 31))
    cv = np.stack([inp["conv_b"].reshape(2, 4, 128), inp["conv_norm_g"], inp["conv_norm_b"]], axis=2)
    sh["conv_v"] = np.ascontiguousarray(cv.transpose(3, 0, 1, 2).reshape(128, 24))
    wr = np.concatenate([inp["w_group"], inp["w_router"].transpose(0, 2, 1, 3).reshape(4, 1024, 32)], axis=2)
    sh["w_route"] = np.ascontiguousarray(wr)
    sh["b_route"] = np.ascontiguousarray(np.concatenate([inp["b_group"], inp["b_router"].reshape(4, 32)], axis=1))
    sh["w_exp_gate"] = inp["w_exp_gate"]
    sh["w_exp_up"] = inp["w_exp_up"]
    sh["w_exp_down"] = inp["w_exp_down"]
    per_core = []
    for i in range(8):
        m = dict(sh)
        b0 = 2 * i
        xt = np.empty((2, 1024, T), f32)
        for s in range(2):
            xt[s, :, :NCTX] = inp["ctx"][b0 + s].T
            xt[s, :, NCTX:] = inp["x"][b0 + s].T
        m["xT"] = xt
        c3 = np.stack([inp["c"][b0], inp["c"][b0 + 1], inp["c_ctx"]], axis=1)
        m["c3"] = np.ascontiguousarray(c3.reshape(8, 128, 3).transpose(1, 0, 2).reshape(128, 24))
        per_core.append(m)
    return per_core


class StopBuild(Exception):
    pass


class Prog:
    def __init__(self, layers=(0, 1, 2, 3), seqs=(0, 1), dbg=None, stop=None):
        self.layers = layers
        self.seqs = seqs
        self.dbg = dbg
        self.stop = stop
        nc = bass.Bass("TRN2", target_bir_lowering=False)
        self.nc = nc
        self.K = Sched(nc)
        self.declare_dram()
        self.alloc()
        self.prologue()
        for s in seqs:
            try:
                self.sequence(s)
            except StopBuild:
                pass
        self.K.emit()

    def declare_dram(self):
        nc = self.nc
        def din(name, shape, dt=F32):
            return nc.dram_tensor(name, list(shape), dt, kind="ExternalInput").ap()
        self.xT = din("xT", [2, 1024, T])
        self.c3 = din("c3", [128, 24])
        self.w_mod = din("w_mod", [4, 1024, 6144])
        self.b_mod = din("b_mod", [128, 192])
        self.ln_g = din("ln_g", [128, 64])
        self.ln_b = din("ln_b", [128, 64])
        self.w_in_ab = din("w_in_ab", [2, 1024, NWIN])
        self.w_out_ab = din("w_out_ab", [2, 1024, 1024])
        self.gains = din("gains", [128, 8])
        self.rope = din("rope", [128, 2, 2048])
        self.dftc = din("dftc", [2048, 2048], BF16)
        self.dfts = din("dfts", [2048, 2048], BF16)
        self.dftc256 = din("dftc256", [256, 256], BF16)
        self.dfts256 = din("dfts256", [256, 256], BF16)
        self.dft64 = din("dft64", [128, 256], BF16)
        self.w_fourier = din("w_fourier", [2, 128, 256])
        self.b_fourier = din("b_fourier", [128, 4])
        self.w_in_cd = din("w_in_cd", [2, 1024, 2048])
        self.w_out_cd = din("w_out_cd", [2, 1024, 1024])
        self.sgu_gb = din("sgu_gb", [2, 2, 512])
        self.w_spT = din("w_spT", [2, 128, 512])
        self.b_sp = din("b_sp", [2, 512])
        self.conv_w = din("conv_w", [128, 248])
        self.conv_v = din("conv_v", [128, 24])
        self.w_route = din("w_route", [4, 1024, 36])
        self.b_route = din("b_route", [4, 36])
        self.w_exp_gate = din("w_exp_gate", [4, 32, 1024, 256])
        self.w_exp_up = din("w_exp_up", [4, 32, 1024, 256])
        self.w_exp_down = din("w_exp_down", [4, 32, 256, 1024])
        self.outT = nc.dram_tensor("outT", [2, 1024, NLAT], F32, kind="ExternalOutput").ap()
        if self.dbg:
            self.dbgT = nc.dram_tensor("dbgT", [1024, T], F32, kind="ExternalOutput").ap()

    def alloc(self):
        K = self.K
        o = ARENA0
        def take(n):
            nonlocal o
            r = o
            o += (n + 31) // 32 * 32
            return r
        self.X = K.sb("X", [128, 8, T], F32, take(8 * T * 4))
        self.MOD = K.sb("MOD", [128, 4, 48, 3], F32, take(4 * 48 * 3 * 4))
        self.BMOD = K.sb("BMOD", [128, 4, 48], F32, take(192 * 4))
        self.LNG = K.sb("LNG", [128, 4, 2, 8], F32, take(256))
        self.LNB = K.sb("LNB", [128, 4, 2, 8], F32, take(256))
        self.GAINS = K.sb("GAINS", [128, 2, 4], F32, take(32))
        self.BF = K.sb("BFc", [128, 2, 2], F32, take(16))
        self.CONVW = K.sb("CONVW", [128, 2, 4, 31], F32, take(248 * 4))
        self.CONVV = K.sb("CONVV", [128, 2, 4, 3], F32, take(96))
        self.C3 = K.sb("C3", [128, 8, 3], F32, take(96))
        self.S3 = K.sb("S3", [128, 8, 3], BF16, take(48))
        self.IDB = K.sb("IDB", [128, 128], BF16, take(256))
        self.IDF = K.sb("IDF", [128, 128], F32, take(512))
        self.ONESF = K.sb("ONESF", [128, 128], F32, take(512))
        self.ONESG = K.sb("ONESG", [128, 128], F32, take(512))
        self.BONES = K.sb("BONES", [128, 128], BF16, take(256))
        self.SEL = K.sb("SEL", [64, 32, 128], BF16, take(32 * 128 * 2))
        self.EPSC = K.sb("EPSC", [128, 4], F32, take(16))
        self.DFT64 = K.sb("DFT64", [128, 2, 128], BF16, take(512))
        self.BRT = K.sb("BRT", [128, 4, 36], F32, take(4 * 36 * 4))
        o = (o + 255) // 256 * 256
        self.SC0 = o
        self.SCN = ARENA1 - o
        S = self.SC0
        self.AT = K.sb("AT", [128, 8, T], BF16, S + 0)
        self.KT = K.sb("KT", [128, 2, T], BF16, S + 36864)
        self.V = K.sb("V", [128, 18, 4, 128], BF16, S + 46080)
        self.HT = [K.sb("HT0", [128, 8, 512], BF16, S + 64512)] * 2
        self.WB = [K.sb(f"WB{i}", [128, 8, 512], BF16, S + 80896 + i * 8192) for i in range(2)]
        self.ROPE = [K.sb(f"ROPE{i}", [128, 2, 512], F32, S + 97280 + i * 4096) for i in range(2)]
        self.ABT = []
        for i_ in range(2):
            b = (S + 105472) if i_ == 0 else (S + 72704)
            b2 = (S + 105472 + 7168) if i_ == 0 else (S + 116736)
            d_ = {}
            d_["SQb"] = K.sb(f"SQb{i_}", [128, 512], BF16, b)
            d_["QG"] = K.sb(f"QG{i_}", [128, 512], F32, b + 1024)
            d_["Q2G"] = K.sb(f"Q2G{i_}", [128, 512], F32, b + 3072)
            d_["SD"] = K.sb(f"SD{i_}", [128, 512], F32, b + 5120)
            d_["T1"] = K.sb(f"T1{i_}", [128, 512], F32, b2)
            d_["T2"] = K.sb(f"T2{i_}", [128, 512], F32, b2 + 2048)
            self.ABT.append(d_)
        self.PT = [K.sb(f"PT{i}", [128, 512], BF16, S + 64512 + i * 1024) for i in range(6)]
        self.RC = [K.sb(f"RC{i}", [64, 512], F32, S + 64512 + 6144 + i * 2048) for i in range(2)]
        self.UV = K.sb("UV", [128, 18, 512], BF16, S + 36864)
        self.DC = K.sb("DC", [128, 16, 512], BF16, S + 55296)
        self.DS = K.sb("DS", [128, 16, 512], BF16, S + 71680)
        self.MX = K.sb("MX", [128, 2, 512], BF16, S + 88064)
        self.WFB = K.sb("WFB", [128, 2, 128], BF16, S + 90112)
        self.VG = K.sb("VG", [128, 18, 512], BF16, S + 36864)
        self.GLU = K.sb("GLU", [128, 4, 2364], BF16, S + 55296)
        self.CHT = [K.sb(f"CHT{i}", [128, 8, 512], BF16, S + 74240 + i * 8192) for i in range(2)]
        self.CWB = [K.sb(f"CWB{i}", [128, 8, 512], BF16, S + 90624 + i * 8192) for i in range(2)]
        self.DG = K.sb("DG", [128, 4, 31, 128], BF16, S + 74240)
        b = S + 107008
        self.CVV = K.sb("CVV", [128, 512], F32, b)
        self.CSG = K.sb("CSG", [128, 512], F32, b + 2048)
        self.GBC = K.sb("GBC", [128, 2, 512], F32, b + 4096)
        self.BST = K.sb("BST", [128, 4, 6], F32, b + 8192)
        self.MV = K.sb("MV", [128, 4, 2], F32, b + 8192 + 96)
        self.RS4 = K.sb("RS4", [128, 4], F32, b + 8192 + 128)
        self.SVt = K.sb("SVt", [128, 512], F32, b + 8448)
        self.WSP = K.sb("WSP", [128, 4, 128], BF16, b + 10496)
        self.BSPb = K.sb("BSPb", [128, 4, 128], F32, b + 11520)
        assert b + 13568 <= S + self.SCN
        self.CM = K.sb("CM", [128, 512], F32, S + 36864)
        self.CV2 = [dict(dw=K.sb(f"cdw{i}", [128, 512], F32, S + 38912 + i * 8192), dsq=K.sb(f"cdsq{i}", [128, 512], F32, S + 40960 + i * 8192),
                         M=K.sb(f"cM{i}", [128, 512], F32, S + 43008 + i * 8192), VV=K.sb(f"cVV{i}", [128, 512], F32, S + 45056 + i * 8192)) for i in range(2)]
        self.WO = K.sb("WO", [128, 8, 1024], BF16, S + 36864)
        b = S + 53248
        self.LSQ = [K.sb(f"LSQ{i}", [128, 512], F32, b + i * 2048) for i in range(2)]
        self.LM = K.sb("LM", [128, 512], F32, b + 4096)
        self.LMS = K.sb("LMS", [128, 512], F32, b + 6144)
        self.LV = K.sb("LV", [128, 512], F32, b + 8192)
        self.LZ = [K.sb(f"LZ{i}", [128, 512], F32, b + 10240 + i * 2048) for i in range(2)]
        self.LM2 = K.sb("LM2", [128, 512], F32, b + 14336)
        self.LMS2 = K.sb("LMS2", [128, 512], F32, b + 16384)
        self.LV2 = K.sb("LV2", [128, 512], F32, b + 18432)
        self.ln_par = 0
        self.H = K.sb("H", [128, 8, T], BF16, S + self.SCN - 36864 - 64)
        self.WG = [K.sb(f"WG{i}", [128, 2, 8, 256], BF16, S + i * 24576) for i in range(2)]
        self.WU = [K.sb(f"WU{i}", [128, 2, 8, 256], BF16, S + i * 24576 + 8192) for i in range(2)]
        self.WD = [K.sb(f"WD{i}", [128, 2, 2, 1024], BF16, S + i * 24576 + 16384) for i in range(2)]
        self.COMBT = K.sb("COMBT", [64, T], BF16, S + 49152)
        self.HID = [K.sb(f"HID{i}", [128, 4, 512], BF16, S + 53760 + i * 4096) for i in range(2)]
        self.SG = [K.sb(f"SG{i}", [128, 512], F32, S + 61952 + i * 2048) for i in range(2)]
        self.TM = [K.sb(f"TM{i}", [128, 512], F32, S + 66048 + i * 2048) for i in range(2)]
        self.HF = K.sb("HF", [128, 8, 128], F32, S + 70144)
        self.WR = K.sb("WR", [128, 8, 36], F32, S + 74240)
        b = S + 75392
        self.RL = K.sb("RL", [128, 36], F32, b)
        self.RT = K.sb("RT", [128, 64], F32, b + 160)
        self.RCMB = K.sb("RCMB", [128, 32], F32, b + 416)
        self.RCB2 = K.sb("RCB2", [128, 64], BF16, b + 544)
        self.RTOP = K.sb("RTOP", [128, 8], F32, b + 672)
        assert S + self.SCN - 36864 - 64 >= b + 1024
        print("scratch bytes", self.SCN, "start", self.SC0)
        self.PS = [K.ps(f"ps{i}") for i in range(8)]

    def dbg_dump(self, src, ncols=T, chunks=8):
        K = self.K
        for c in range(chunks):
            K.dma("pool", self.dbgT[c * 128:(c + 1) * 128, 0:ncols], src[:, c, 0:ncols])

    def prologue(self):
        K = self.K
        nc = self.nc
        K.dma("sp", self.C3[:].rearrange("p c n -> p (c n)"), self.c3[:, :])
        K.dma("sp", self.BMOD[:].rearrange("p l j -> p (l j)"), self.b_mod[:, :])
        K.dma("sp", self.LNG[:].rearrange("p l s c -> p (l s c)"), self.ln_g[:, :])
        K.dma("sp", self.LNB[:].rearrange("p l s c -> p (l s c)"), self.ln_b[:, :])
        K.dma("sp", self.GAINS[:].rearrange("p i k -> p (i k)"), self.gains[:, :])
        K.dma("sp", self.BF[:].rearrange("p i k -> p (i k)"), self.b_fourier[:, :])
        K.dma("sp", self.CONVW[:].rearrange("p i c k -> p (i c k)"), self.conv_w[:, :])
        K.dma("sp", self.CONVV[:].rearrange("p i c k -> p (i c k)"), self.conv_v[:, :])
        K.dma("sp", self.DFT64[:].rearrange("p a b -> p (a b)"), self.dft64[:, :])
        for l in range(4):
            K.dma("sp", self.BRT[:, l, :], self.b_route[l:l + 1, :].partition_broadcast(128))
        K.strict = True
        K.memset(self.ONESF[:], 1.0 / 1024.0)
        K.memset(self.ONESG[:], 1.0 / 128.0)
        K.memset(self.EPSC[:, 0:1], EPS / (ALPHA * ALPHA))
        K.memset(self.EPSC[:, 1:2], EPS)
        K.memset(self.EPSC[:, 2:3], 1.0)
        K.memset(self.BONES[:], 0.0)
        K.memset(self.BONES[0:64, 0:64], 1.0 / 64.0)
        K.memset(self.BONES[64:128, 64:128], 1.0 / 64.0)
        K.memset(self.IDF[:], 0.0, eng="pool")
        K.op("pool", lambda e: e.affine_select(self.IDF[:], self.IDF[:], [[-1, 128]], ALU.not_equal, 1.0, base=0, channel_multiplier=1),
             r=(self.IDF[:],), w=(self.IDF[:],))
        K.copy(self.IDB[:], self.IDF[:])
        K.memset(self.SEL[:], 0.0)
        for e in range(32):
            K.copy(self.SEL[0:64, e, :], self.IDB[0:64, e:e + 1].to_broadcast([64, 128]))
            K.tt(self.SEL[0:64, e, :], self.SEL[0:64, e, :], self.IDB[0:64, 32 + e:33 + e].to_broadcast([64, 128]), ALU.add)
        K.act(self.S3[:], self.C3[:], AF.Silu)
        K.strict = False
        WB = [K.sb(f"WMB{i}", [128, 8, 1024], BF16, self.SC0 + i * 16384) for i in range(2)]
        it = 0
        for l in self.layers:
            pm = self.PS[0]
            for blk in range(6):
                wb = WB[it % 2]
                it += 1
                K.dma("pool", wb[:], self.w_mod[l, :, blk * 1024:(blk + 1) * 1024].rearrange("(c p) n -> p c n", p=128))
                for jj in range(8):
                    j = blk * 8 + jj
                    for k in range(8):
                        K.mm(pm[:, j * 3:(j + 1) * 3], wb[:, k, jj * 128:(jj + 1) * 128], self.S3[:, k, :], start=(k == 0), stop=(k == 7))
            K.strict = True
            K.tt(self.MOD[:, l, :, :], pm[:, 0:144].rearrange("p (j n) -> p j n", n=3),
                 self.BMOD[:, l, :].unsqueeze(2).to_broadcast([128, 48, 3]), ALU.add)
            K.ts(self.MOD[:, l, 8:16, :], self.MOD[:, l, 8:16, :], 1.0, None, ALU.add)
            K.ts(self.MOD[:, l, 32:40, :], self.MOD[:, l, 32:40, :], 1.0, None, ALU.add)
            K.ts(self.MOD[:, l, 16:24, :], self.MOD[:, l, 16:24, :], 1.0 / ALPHA, None, ALU.mult)
            K.ts(self.MOD[:, l, 40:48, :], self.MOD[:, l, 40:48, :], 1.0 / ALPHA, None, ALU.mult)
            K.strict = False

    def mod(self, l, part, c, n):
        return self.MOD[:, l, part * 8 + c, n:n + 1]

    def sequence(self, s):
        K = self.K
        for (t0, sz) in TILES:
            K.dma("sp", self.X[:, :, t0:t0 + sz], self.xT[s, :, t0:t0 + sz].rearrange("(c p) t -> p c t", p=128))
        for l in self.layers:
            ctx_out = l < 2
            ctx_kv = (l == 2)
            if l % 2 == 0:
                self.mixer_ab(s, l, ctx_out)
            else:
                self.mixer_cd(s, l, ctx_out)
            if self.stop == ("mix", l):
                self.dbg_dump(self.AT)
                return
            self.wout_ln1(s, l, ctx_out)
            if self.stop == ("ln1", l):
                self.dbg_dump(self.X)
                return
            self.moe(s, l, ctx_out)
            if self.stop == ("moe", l):
                self.dbg_dump(self.X)
                return
        for c in range(8):
            K.dma("sp", self.outT[s, c * 128:(c + 1) * 128, :], self.X[:, c, NCTX:T])

    def ln_tile(self, l, st, t0, sz, epscol):
        K = self.K
        X = self.X
        par = self.ln_par
        self.ln_par = 1 - par
        s1, s2 = self.PS[6 - 2 * par], self.PS[7 - 2 * par]
        for c in range(8):
            sq = self.LSQ[c % 2]
            K.act(sq[:, :sz], X[:, c, t0:t0 + sz], AF.Square)
            K.mm(s1[:, :sz], self.ONESF[:], X[:, c, t0:t0 + sz], start=(c == 0), stop=(c == 7))
            K.mm(s2[:, :sz], self.ONESF[:], sq[:, :sz], start=(c == 0), stop=(c == 7))
        import os
        LNCUT = int(os.environ.get("LNCUT", "9"))
        if LNCUT < 2:
            return
        M, MS, VV = (self.LM, self.LMS, self.LV) if par == 0 else (self.LM2, self.LMS2, self.LV2)
        K.copy(M[:, :sz], s1[:, :sz], eng="act")
        K.act(MS[:, :sz], s1[:, :sz], AF.Square)
        K.tt(VV[:, :sz], s2[:, :sz], MS[:, :sz], ALU.subtract)
        self.rstd(VV[:, :sz], VV[:, :sz], epscol)
        if LNCUT < 3:
            return
        for c in range(8):
            z = self.LZ[c % 2]
            ze = "dve"
            K.tt(z[:, :sz], X[:, c, t0:t0 + sz], M[:, :sz], ALU.subtract, eng=ze)
            K.tt(z[:, :sz], z[:, :sz], VV[:, :sz], ALU.mult, eng=ze)
            K.act(X[:, c, t0:t0 + sz], z[:, :sz], AF.Identity, bias=self.LNB[:, l, st, c:c + 1], scale=self.LNG[:, l, st, c:c + 1])

    def wout_ln1(self, s, l, ctx_out):
        K = self.K
        X, AT, WO, H = self.X, self.AT, self.WO, self.H
        i = l // 2
        wsrc = self.w_out_ab if l % 2 == 0 else self.w_out_cd
        K.dma("pool", WO[:], wsrc[i].rearrange("(c p) n -> p c n", p=128))
        for ti, (t0, sz) in enumerate(TILES):
            if ti == 0 and not ctx_out:
                continue
            n = 2 if ti == 0 else s
            for oc in range(8):
                ps = self.PS[oc % 4]
                for k in range(8):
                    K.mm(ps[:, :sz], WO[:, k, oc * 128:(oc + 1) * 128], AT[:, k, t0:t0 + sz], start=(k == 0), stop=(k == 7))
                K.stt(X[:, oc, t0:t0 + sz], ps[:, :sz], self.mod(l, 2, oc, n), X[:, oc, t0:t0 + sz], ALU.mult, ALU.add)
            import os
            if os.environ.get("NOLN"):
                continue
            self.ln_tile(l, 0, t0, sz, 0)
            if os.environ.get("NOH"):
                continue
            for c in range(8):
                if True:
                    K.ts(H[:, c, t0:t0 + sz], X[:, c, t0:t0 + sz], self.mod(l, 4, c, n), self.mod(l, 3, c, n), ALU.mult, ALU.add)
                else:
                    K.act(H[:, c, t0:t0 + sz], X[:, c, t0:t0 + sz], AF.Identity, bias=self.mod(l, 3, c, n), scale=self.mod(l, 4, c, n))

    def mixer_ab(self, s, l, ctx_out):
        K = self.K
        X, AT, KT, V = self.X, self.AT, self.KT, self.V
        PS = self.PS
        i = l // 2
        G = self.GAINS
        K.memset(V[:, :, :, 64:128], 1.0)
        wit = 0
        for ti, (t0, sz) in enumerate(TILES):
            is_ctx = (ti == 0)
            n = 2 if is_ctx else s
            ht = self.HT[ti % 2]
            for c in range(8):
                K.act(ht[:, c, :sz], X[:, c, t0:t0 + sz], AF.Identity, bias=self.mod(l, 0, c, n), scale=self.mod(l, 1, c, n))
            rp = self.ROPE[ti % 2]
            if not is_ctx:
                K.dma("sp", rp[:, :, :sz], self.rope[:, :, t0 - NCTX:t0 - NCTX + sz])
            import os
            CUT = int(os.environ.get("CUT", "99"))
            for blk in range(5):
                if is_ctx and blk < 3 and not ctx_out:
                    continue
                if blk >= CUT:
                    continue
                wb = self.WB[wit % 2]
                wit += 1
                K.dma("pool", wb[:], self.w_in_ab[i, :, blk * 512:(blk + 1) * 512].rearrange("(c p) n -> p c n", p=128))

                def proj(ps, col0, ncol=128):
                    for k in range(8):
                        K.mm(ps[0:ncol, :sz], wb[:, k, col0:col0 + ncol], ht[:, k, :sz], start=(k == 0), stop=(k == 7))

                if blk < 4:
                    for cc in range(2):
                        if blk < 3:
                            j = blk * 2 + cc
                            dest = AT[:, j, t0:t0 + sz]
                            g0 = 0
                        else:
                            j = cc
                            dest = KT[:, j, t0:t0 + sz]
                            g0 = 2
                        pq, pq2, ss = PS[cc], PS[2 + cc], PS[4 + cc]
                        tset = self.ABT[cc]
                        SQb, QG, Q2G, SD, T1, T2 = tset["SQb"], tset["QG"], tset["Q2G"], tset["SD"], tset["T1"], tset["T2"]
                        proj(pq, cc * 256)
                        if not is_ctx:
                            proj(pq2, cc * 256 + 128)
                        K.act(SQb[:, :sz], pq[:, :sz], AF.Square)
                        K.act(QG[:, :sz], pq[:, :sz], AF.Identity, scale=G[:, i, g0:g0 + 1])
                        K.mm(ss[:, :sz], self.BONES[:], SQb[:, :sz])
                        self.rstd(SD[:, :sz], ss[:, :sz], 1)
                        if not is_ctx:
                            K.act(Q2G[:, :sz], pq2[:, :sz], AF.Identity, scale=G[:, i, g0 + 1:g0 + 2])
                            K.tt(T1[:, :sz], QG[:, :sz], rp[:, 0, :sz], ALU.mult)
                            K.tt(T2[:, :sz], Q2G[:, :sz], rp[:, 1, :sz], ALU.mult, eng="pool")
                            K.tt(T1[:, :sz], T1[:, :sz], T2[:, :sz], ALU.add)
                            K.tt(dest, T1[:, :sz], SD[:, :sz], ALU.mult)
                        else:
                            K.tt(dest, QG[:, :sz], SD[:, :sz], ALU.mult)
                else:
                    if ((not is_ctx) or ctx_out) and os.environ.get("NOF") is None:
                        for fc in range(2):
                            ps = PS[fc]
                            proj(ps, fc * 128)
                            K.copy(AT[:, 6 + fc, t0:t0 + sz], ps[:, :sz], eng="act")
                    for up in range(sz // 256 if os.environ.get("NOV") is None else 0):
                        tc = (t0 + up * 256) // 128
                        ps = PS[6 + up % 2]
                        for uu in range(2):
                            u = up * 2 + uu
                            o = uu * 256
                            for k in range(8):
                                K.mm(ps[:, o:o + 256], ht[:, k, u * 128:(u + 1) * 128], wb[:, k, 256:512], start=(k == 0), stop=(k == 7))
                        K.copy(V[:, tc:tc + 2, :, 0:64], ps[:, 0:512].rearrange("p (u g d) -> p u g d", u=2, d=64))
        if self.stop == ("proj", l):
            self.dbg_dump(self.AT)
            raise StopBuild()
        calls = []
        if ctx_out:
            calls.append((0, 256, 2))
        for (t0, sz) in TILES[1:]:
            calls.append((t0, sz, 18))
        cn = 0
        for j in range(6):
            kc = j // 3
            gA, gB = 2 * kc, 2 * kc + 1
            for (t0, sz, nk) in calls:
                OA, OB = PS[4 + 2 * (cn % 2)], PS[5 + 2 * (cn % 2)]
                cn += 1
                pts = {}
                for kk in range(nk + 1):
                    if kk < nk:
                        SA, SB = PS[(kk % 2) * 2], PS[(kk % 2) * 2 + 1]
                        pa, pb = self.PT[(kk % 3) * 2], self.PT[(kk % 3) * 2 + 1]
                        K.mm(SA[:, :sz], KT[0:64, kc, kk * 128:(kk + 1) * 128], AT[0:64, j, t0:t0 + sz])
                        K.mm(SB[:, :sz], KT[64:128, kc, kk * 128:(kk + 1) * 128], AT[64:128, j, t0:t0 + sz])
                        K.act(pa[:, :sz], SA[:, :sz], AF.Exp, scale=0.125)
                        K.act(pb[:, :sz], SB[:, :sz], AF.Exp, scale=0.125)
                        pts[kk] = (pa, pb)
                    if kk >= 1:
                        pa, pb = pts.pop(kk - 1)
                        K.mm(OA[:, :sz], V[:, kk - 1, gA, :], pa[:, :sz], start=(kk == 1), stop=(kk == nk))
                        K.mm(OB[:, :sz], V[:, kk - 1, gB, :], pb[:, :sz], start=(kk == 1), stop=(kk == nk))
                K.recip(self.RC[0][0:64, :sz], OA[64:128, :sz])
                K.tt(AT[0:64, j, t0:t0 + sz], OA[0:64, :sz], self.RC[0][0:64, :sz], ALU.mult)
                K.recip(self.RC[1][0:64, :sz], OB[64:128, :sz])
                K.tt(AT[64:128, j, t0:t0 + sz], OB[0:64, :sz], self.RC[1][0:64, :sz], ALU.mult)
        if self.stop == ("attn", l):
            self.dbg_dump(self.AT)
            raise StopBuild()
        UV, DC, DS, MX, WFB = self.UV, self.DC, self.DS, self.MX, self.WFB
        K.dma("pool", WFB[:].rearrange("p a b -> p (a b)"), self.w_fourier[i, :, :])
        tcs = list(range(2, 18)) + ([0, 1] if ctx_out else [])
        for q, tc in enumerate(tcs):
            ps = PS[q % 2]
            for cs in range(2):
                for fc in range(2):
                    o = (cs * 2 + fc) * 128
                    K.mm(ps[:, o:o + 128], AT[:, 6 + fc, tc * 128:(tc + 1) * 128], self.DFT64[:, cs, :])
            K.copy(UV[:, tc, :], ps[:, :], eng=("act" if q % 2 else "dve"))
        jobs = [(256 + nt * 512, 512, 2, 16, nt) for nt in range(4)]
        if ctx_out:
            jobs.append((0, 256, 0, 2, -1))
        for (t0, sz, tc0, ntc, nt) in jobs:
            if nt >= 0:
                K.dma("sp", DC[:, :, :], self.dftc[:, nt * 512:(nt + 1) * 512].rearrange("(c p) n -> p c n", p=128))
                K.dma("sp", DS[:, :, :], self.dfts[:, nt * 512:(nt + 1) * 512].rearrange("(c p) n -> p c n", p=128))
            else:
                K.dma("sp", DC[:, 0:2, 0:256], self.dftc256[:, :].rearrange("(c p) n -> p c n", p=128))
                K.dma("sp", DS[:, 0:2, 0:256], self.dfts256[:, :].rearrange("(c p) n -> p c n", p=128))
            for fc in range(2):
                ps = PS[2 + fc]
                for a in range(ntc):
                    K.mm(ps[:, :sz], UV[:, tc0 + a, fc * 128:(fc + 1) * 128], DC[:, a, :sz], start=(a == 0), stop=False)
                for a in range(ntc):
                    K.mm(ps[:, :sz], UV[:, tc0 + a, 256 + fc * 128:256 + (fc + 1) * 128], DS[:, a, :sz], start=False, stop=(a == ntc - 1))
                K.copy(MX[:, fc, :sz], ps[:, :sz], eng="act")
                ps2 = PS[4 + fc]
                K.mm(ps2[:, :sz], WFB[:, fc, :], MX[:, fc, :sz])
                K.act(AT[:, 6 + fc, t0:t0 + sz], ps2[:, :sz], AF.Identity, bias=self.BF[:, i, fc:fc + 1])

    def rstd(self, out, in_, epscol):
        K = self.K
        K.act(out, in_, AF.Ln, bias=self.EPSC[:, epscol:epscol + 1])
        K.act(out, out, AF.Exp, scale=-0.5)

    def sigmoid(self, out, in_, scale=1.0):
        K = self.K
        K.act(out, in_, AF.Exp, scale=-scale)
        K.act(out, out, AF.Ln, bias=self.EPSC[:, 2:3])
        K.act(out, out, AF.Exp, scale=-1.0)

    def gelu(self, out, ps, tmp, tmp2):
        K = self.K
        K.act(tmp, ps, AF.Square)
        K.ts(tmp, tmp, 0.044715, 1.0, ALU.mult, ALU.add)
        K.tt(tmp, tmp, ps, ALU.mult)
        self.sigmoid(tmp2, tmp, 1.5957691216057308)
        K.tt(out, tmp2, ps, ALU.mult)

    def mixer_cd(self, s, l, ctx_out):
        K = self.K
        X, YT, VG, GLU = self.X, self.AT, self.VG, self.GLU
        PS = self.PS
        i = l // 2
        K.dma("sp", self.GBC[:, 0, :], self.sgu_gb[i, 0:1, :].partition_broadcast(128))
        K.dma("sp", self.GBC[:, 1, :], self.sgu_gb[i, 1:2, :].partition_broadcast(128))
        K.memset(GLU[:, :, 0:15], 0.0)
        K.memset(GLU[:, :, 271:301], 0.0)
        K.memset(GLU[:, :, 2349:2364], 0.0)
        def gcol(t0):
            return 15 + t0 if t0 < NCTX else 301 + (t0 - NCTX)
        wit = 0
        for ti, (t0, sz) in enumerate(TILES):
            is_ctx = (ti == 0)
            if is_ctx and not ctx_out:
                continue
            n = 2 if is_ctx else s
            ht = self.CHT[ti % 2]
            for c in range(8):
                K.act(ht[:, c, :sz], X[:, c, t0:t0 + sz], AF.Identity, bias=self.mod(l, 0, c, n), scale=self.mod(l, 1, c, n))
            for blk in range(4):
                wb = self.CWB[wit % 2]
                wit += 1
                K.dma("pool", wb[:], self.w_in_cd[i, :, blk * 512:(blk + 1) * 512].rearrange("(c p) n -> p c n", p=128))
                if blk == 0:
                    for c in range(4):
                        ps = PS[c % 2]
                        for k in range(8):
                            K.mm(ps[:, :sz], wb[:, k, c * 128:(c + 1) * 128], ht[:, k, :sz], start=(k == 0), stop=(k == 7))
                        self.gelu(YT[:, c, t0:t0 + sz], ps[:, :sz], self.CVV[:, :sz], self.CSG[:, :sz])
                elif blk == 1:
                    for u in range(sz // 128):
                        tc = (t0 + u * 128) // 128
                        ps = PS[2 + u % 2]
                        for k in range(8):
                            K.mm(ps[:, :], ht[:, k, u * 128:(u + 1) * 128], wb[:, k, :], start=(k == 0), stop=(k == 7))
                        vv = self.SVt
                        self.gelu(vv[:, :], ps[:, :], self.CVV[:, :], self.CSG[:, :])
                        K.strict = True
                        for g in range(4):
                            K.op("dve", lambda e, g=g: e.bn_stats(self.BST[:, g, :], vv[:, g * 128:(g + 1) * 128]),
                                 r=(vv[:, g * 128:(g + 1) * 128],), w=(self.BST[:, g, :],))
                            K.op("dve", lambda e, g=g: e.bn_aggr(self.MV[:, g, :], self.BST[:, g, :]),
                                 r=(self.BST[:, g, :],), w=(self.MV[:, g, :],))
                        self.rstd(self.RS4[:, :], self.MV[:, :, 1], 1)
                        for g in range(4):
                            K.ts(vv[:, g * 128:(g + 1) * 128], vv[:, g * 128:(g + 1) * 128], self.MV[:, g, 0:1], self.RS4[:, g:g + 1], ALU.subtract, ALU.mult)
                        K.strict = False
                        K.tt(vv[:, :], vv[:, :], self.GBC[:, 0, :], ALU.mult)
                        K.tt(VG[:, tc, :], vv[:, :], self.GBC[:, 1, :], ALU.add)
                else:
                    for cc in range(2):
                        c = (blk - 2) * 2 + cc
                        pa, pg = PS[4 + cc], PS[6 + cc]
                        for k in range(8):
                            K.mm(pa[:, :sz], wb[:, k, cc * 256:cc * 256 + 128], ht[:, k, :sz], start=(k == 0), stop=(k == 7))
                        for k in range(8):
                            K.mm(pg[:, :sz], wb[:, k, cc * 256 + 128:cc * 256 + 256], ht[:, k, :sz], start=(k == 0), stop=(k == 7))
                        self.sigmoid(self.CSG[:, :sz], pg[:, :sz])
                        gc = gcol(t0)
                        K.tt(GLU[:, c, gc:gc + sz], pa[:, :sz], self.CSG[:, :sz], ALU.mult)
        if self.stop == ("cdproj", l):
            for c in range(4):
                K.dma("pool", self.dbgT[c * 128:(c + 1) * 128, 0:T], self.VG[:].rearrange("p t f -> p (t f)")[:, c * T:(c + 1) * T])
            raise StopBuild()
        DG = self.DG
        for c in range(4):
            for k in range(31):
                K.ts(DG[:, c, k, :], self.IDB[:], self.CONVW[:, i, c, k:k + 1], None, ALU.mult, eng="dve")
        K.dma("pool", self.WSP[:].rearrange("p g q -> p (g q)"), self.w_spT[i, :, :])
        K.dma("sp", self.BSPb[:].rearrange("p g q -> p (g q)"), self.b_sp[i:i + 1, :].partition_broadcast(128))
        groups = ([[0, 1]] if ctx_out else []) + [[2 + 4 * a + b for b in range(4)] for a in range(4)]
        q = 0
        for g in range(4):
            for grp in groups:
                ps = PS[q % 2]
                q += 1
                na = len(grp)
                for a, tc in enumerate(grp):
                    K.mm(ps[:, a * 128:(a + 1) * 128], VG[:, tc, g * 128:(g + 1) * 128], self.WSP[:, g, :])
                c0 = grp[0] * 128
                K.tt(self.SVt[:, :na * 128].rearrange("p (a q) -> p a q", q=128), ps[:, :na * 128].rearrange("p (a q) -> p a q", q=128),
                     self.BSPb[:, g, :].unsqueeze(1).to_broadcast([128, na, 128]), ALU.add)
                K.tt(YT[:, g, c0:c0 + na * 128], self.SVt[:, :na * 128], YT[:, g, c0:c0 + na * 128], ALU.mult)
        if self.stop == ("cdsp", l):
            self.dbg_dump(self.AT)
            raise StopBuild()
        DG = self.DG
        q = 0
        CV = int(os.environ.get("CVCUT", "9"))
        for ti, (t0, sz) in enumerate(TILES):
            if ti == 0 and not ctx_out:
                continue
            if CV < 2:
                continue
            gc = gcol(t0)
            for c in range(4):
                ps = PS[2 + q % 2]
                q += 1
                for k in range(31):
                    K.mm(ps[:, :sz], DG[:, c, k, :], GLU[:, c, gc + k - 15:gc + k - 15 + sz], start=(k == 0), stop=(k == 30))
                cv = self.CV2[q % 2]
                dw, dsq = cv["dw"], cv["dsq"]
                K.act(dw[:, :sz], ps[:, :sz], AF.Identity, bias=self.CONVV[:, i, c, 0:1])
                if CV < 3:
                    continue
                K.act(dsq[:, :sz], ps[:, :sz], AF.Square, bias=self.CONVV[:, i, c, 0:1])
                s1, s2 = PS[4 + 2 * (q % 2)], PS[5 + 2 * (q % 2)]
                K.mm(s1[:, :sz], self.ONESG[:], dw[:, :sz])
                K.mm(s2[:, :sz], self.ONESG[:], dsq[:, :sz])
                M, VV = cv["M"], cv["VV"]
                K.copy(M[:, :sz], s1[:, :sz], eng="act")
                K.act(dsq[:, :sz], s1[:, :sz], AF.Square)
                K.tt(VV[:, :sz], s2[:, :sz], dsq[:, :sz], ALU.subtract)
                self.rstd(VV[:, :sz], VV[:, :sz], 1)
                if CV < 4:
                    continue
                K.tt(dw[:, :sz], dw[:, :sz], M[:, :sz], ALU.subtract)
                K.tt(dw[:, :sz], dw[:, :sz], VV[:, :sz], ALU.mult)
                K.ts(dw[:, :sz], dw[:, :sz], self.CONVV[:, i, c, 1:2], self.CONVV[:, i, c, 2:3], ALU.mult, ALU.add)
                if os.environ.get("NOSILU"):
                    K.copy(YT[:, 4 + c, t0:t0 + sz], dw[:, :sz])
                else:
                    self.sigmoid(dsq[:, :sz], dw[:, :sz])
                    K.tt(YT[:, 4 + c, t0:t0 + sz], dw[:, :sz], dsq[:, :sz], ALU.mult)

    def moe(self, s, l, ctx_out):
        K = self.K
        X, H, PS = self.X, self.H, self.PS
        RT, RL = self.RT, self.RL
        K.dma("sp", self.WR[:], self.w_route[l].rearrange("(c p) n -> p c n", p=128))
        tiles = [(ti, t0, sz) for ti, (t0, sz) in enumerate(TILES) if not (ti == 0 and not ctx_out)]
        ptb = PS[1][:, 0:64].bitcast(BF16)

        def route_chunk(tc):
                c0 = tc * 128
                n = 2 if tc < 2 else s
                for c in range(8):
                    K.act(self.HF[:, c, :], X[:, c, c0:c0 + 128], AF.Identity, bias=self.mod(l, 3, c, n), scale=self.mod(l, 4, c, n))
                pl = PS[0]
                for c in range(8):
                    K.mm(pl[:, 0:36], self.HF[:, c, :], self.WR[:, c, :], start=(c == 0), stop=(c == 7))
                K.tt(RL[:], pl[:, 0:36], self.BRT[:, l, :], ALU.add)
                K.strict = True
                gmax, ngmax, gsum, gw = RT[:, 0:1], RT[:, 1:2], RT[:, 2:3], RT[:, 3:4]
                OH, ge = RT[:, 4:8], RT[:, 8:12]
                d, e2, den, r = RT[:, 12:13], RT[:, 13:14], RT[:, 14:15], RT[:, 15:16]
                esel, mask1, mask2 = RT[:, 16:24], RT[:, 24:32], RT[:, 32:40]
                w1, w2, wsel = RT[:, 40:41], RT[:, 41:42], RT[:, 48:56]
                K.op("dve", lambda e: e.reduce_max(gmax, RL[:, 0:4], AX.X), r=(RL[:, 0:4],), w=(gmax,))
                K.ts(OH, RL[:, 0:4], gmax, None, ALU.is_equal)
                K.ts(ngmax, gmax, -1.0, None, ALU.mult)
                K.act(ge, RL[:, 0:4], AF.Exp, bias=ngmax, accum_out=gsum)
                K.recip(gw, gsum)
                K.ts(esel, RL[:, 4:12], OH[:, 0:1], None, ALU.mult)
                for g in range(1, 4):
                    K.stt(esel, RL[:, 4 + 8 * g:12 + 8 * g], OH[:, g:g + 1], esel, ALU.mult, ALU.add)
                top = self.RTOP[:, 0:8]
                K.op("dve", lambda e: e.max(top, esel), r=(esel,), w=(top,))
                m1, m2 = top[:, 0:1], top[:, 1:2]
                K.ts(mask1, esel, m1, None, ALU.is_equal)
                K.ts(mask2, esel, m2, None, ALU.is_equal)
                K.tt(d, m2, m1, ALU.subtract)
                K.act(e2, d, AF.Exp)
                K.ts(den, e2, 1.0, None, ALU.add)
                K.recip(r, den)
                K.tt(w1, r, gw, ALU.mult)
                K.tt(w2, w1, e2, ALU.mult)
                K.ts(wsel, mask1, w1, None, ALU.mult)
                K.stt(wsel, mask2, w2, wsel, ALU.mult, ALU.add)
                K.tt(self.RCMB[:].rearrange("p (g j) -> p g j", j=8), OH.unsqueeze(2).to_broadcast([128, 4, 8]),
                     wsel.unsqueeze(1).to_broadcast([128, 4, 8]), ALU.mult)
                K.copy(self.RCB2[:, 0:32], self.RCMB[:])
                K.tt(self.RCB2[:, 32:64], self.RCMB[:], self.RCB2[:, 0:32], ALU.subtract)
                K.strict = False
                K.transpose(ptb[0:64, 0:128], self.RCB2[:, 0:64], self.IDB[:])
                K.copy(self.COMBT[0:64, c0:c0 + 128], ptb[0:64, 0:128])

        jobs = []
        for ep in range(16):
            for (ti, t0, sz) in tiles:
                jobs.append((ep, ti, t0, sz))
        q = 0
        pq = 0

        def load_w(ep):
            slot = ep % 2
            WG, WU, WD = self.WG[slot], self.WU[slot], self.WD[slot]
            for e2_ in range(2):
                e = ep * 2 + e2_
                K.dma("pool", WG[:, e2_, :, :], self.w_exp_gate[l, e].rearrange("(c p) f -> p c f", p=128))
                K.dma("pool", WU[:, e2_, :, :], self.w_exp_up[l, e].rearrange("(c p) f -> p c f", p=128))
                K.dma("pool", WD[:, e2_, :, :], self.w_exp_down[l, e].rearrange("(c p) f -> p c f", p=128))

        def gu(j):
            nonlocal q
            ep, ti, t0, sz = jobs[j]
            slot = ep % 2
            WG, WU = self.WG[slot], self.WU[slot]
            hid = self.HID[j % 2]
            for e2_ in range(2):
                e = ep * 2 + e2_
                cb = PS[4 + e2_]
                K.mm(cb[:, :sz], self.SEL[0:64, e, :], self.COMBT[0:64, t0:t0 + sz])
                for fcn in range(2):
                    pg, pu = PS[(q % 2) * 2], PS[(q % 2) * 2 + 1]
                    sg, tm = self.SG[q % 2], self.TM[q % 2]
                    q += 1
                    for k in range(8):
                        K.mm(pg[:, :sz], WG[:, e2_, k, fcn * 128:(fcn + 1) * 128], H[:, k, t0:t0 + sz], start=(k == 0), stop=(k == 7))
                    for k in range(8):
                        K.mm(pu[:, :sz], WU[:, e2_, k, fcn * 128:(fcn + 1) * 128], H[:, k, t0:t0 + sz], start=(k == 0), stop=(k == 7))
                    K.act(sg[:, :sz], pg[:, :sz], AF.Silu)
                    K.tt(tm[:, :sz], pu[:, :sz], sg[:, :sz], ALU.mult)
                    K.tt(hid[:, e2_ * 2 + fcn, :sz], tm[:, :sz], cb[:, :sz], ALU.mult)

        def down(j):
            nonlocal pq
            ep, ti, t0, sz = jobs[j]
            n = 2 if ti == 0 else s
            WD = self.WD[ep % 2]
            hid = self.HID[j % 2]
            for oc in range(8):
                py = PS[6 + pq % 2]
                pq += 1
                idx = 0
                for e2_ in range(2):
                    for fcn in range(2):
                        K.mm(py[:, :sz], WD[:, e2_, fcn, oc * 128:(oc + 1) * 128], hid[:, e2_ * 2 + fcn, :sz], start=(idx == 0), stop=(idx == 3))
                        idx += 1
                K.stt(X[:, oc, t0:t0 + sz], py[:, :sz], self.mod(l, 5, oc, n), X[:, oc, t0:t0 + sz], ALU.mult, ALU.add)

        for j, (ep, ti, t0, sz) in enumerate(jobs):
            if ti == tiles[0][0]:
                load_w(ep)
            if ep == 0:
                for tc in range(t0 // 128, (t0 + sz) // 128):
                    route_chunk(tc)
            gu(j)
            if j >= 1:
                down(j - 1)
        down(len(jobs) - 1)
        for (ti, t0, sz) in tiles:
            self.ln_tile(l, 1, t0, sz, 0)


def kernel(**inputs):
    inp = {k: np.asarray(v) for k, v in inputs.items()}
    maps = host_prep(inp)
    p = Prog()
    res = run_bass_kernel_spmd(p.nc, maps, core_ids=list(range(8)))
    out = np.empty((16, NLAT, D), np.float32)
    for i in range(8):
        o = np.asarray(res.results[i]["outT"])
        for s in range(2):
            out[2 * i + s] = o[s].T
    return out
```

```python
import numpy as np
import concourse.bass as bass
import concourse.mybir as mybir

F32 = mybir.dt.float32
BF16 = mybir.dt.bfloat16
AF = mybir.ActivationFunctionType
ALU = mybir.AluOpType
AX = mybir.AxisListType

STRICT_ALL = True
SB_PAGE = 256
PS_PAGE = 2048
ARENA0 = 16640
ARENA1 = 229376 - 64
ESZ = {F32: 4, BF16: 2}


class Op:
    __slots__ = ("eng", "fn", "deps", "dma", "sig", "sem", "target", "idx", "extra_wait", "strict")

    def __init__(self, eng, fn, dma):
        self.eng = eng
        self.fn = fn
        self.dma = dma
        self.deps = set()
        self.sig = False
        self.sem = None
        self.target = 0
        self.idx = 0
        self.extra_wait = None


class Sched:
    ENGS = ("pe", "act", "dve", "pool", "sp")

    def __init__(self, nc):
        self.nc = nc
        self.ops = []
        self.base = {}
        self.lastw = {}
        self.readers = {}
        self.dma_count = {"sp": 0, "pool": 0, "act": 0}
        self.dma_hist = {"sp": [], "pool": [], "act": []}
        self.POOLSZ = {"sp": 16, "pool": 16, "act": 8}
        self.npsum = 0
        self.strict = False

    def sb(self, name, shape, dtype, offset):
        assert offset >= ARENA0 and offset % 32 == 0, (name, offset)
        nbytes = int(np.prod(shape[1:])) * ESZ[dtype]
        assert offset + nbytes <= ARENA1, (name, offset, nbytes)
        h = self.nc.alloc_sbuf_tensor_at(name, list(shape), dtype, offset=offset)
        self.base[h.name] = ("sb", offset)
        return h

    def ps(self, name, shape=(128, 512), dtype=F32):
        h = self.nc.alloc_psum_tensor(name, list(shape), dtype)
        self.base[h.name] = ("ps", self.npsum * 2048)
        self.npsum += 1
        return h

    def pages(self, ap):
        tn = ap.tensor.name
        if tn not in self.base:
            return ()
        space, base = self.base[tn]
        esz = ESZ.get(ap.dtype, None)
        if esz is None:
            esz = ap.nbytes() // max(1, ap.size())
        pat = ap.ap
        pstep, pn = pat[0]
        off = int(ap.offset)
        p0 = off // pstep if pstep > 0 else 0
        foff = off - p0 * pstep
        ext = 0
        for st, cnt in pat[1:]:
            ext += (cnt - 1) * abs(st)
        b0 = base + foff * esz
        b1 = base + (foff + ext + 1) * esz
        pg = SB_PAGE if space == "sb" else PS_PAGE
        q0 = p0 // 32
        q1 = (p0 + pn - 1) // 32
        out = []
        for pgi in range(b0 // pg, (b1 - 1) // pg + 1):
            for q in range(q0, q1 + 1):
                out.append((space, pgi, q))
        return out

    def op(self, eng, fn, r=(), w=(), dma=False):
        o = Op(eng, fn, dma)
        o.strict = self.strict
        i = len(self.ops)
        o.idx = i
        deps = o.deps
        r = list(r)
        w = list(w)
        for ap in list(r):
            if ap is None or isinstance(ap, (int, float)):
                continue
            if self.base.get(ap.tensor.name, ("", 0))[0] == "ps":
                w.append(ap)
                continue
            for k in self.pages(ap):
                lw = self.lastw.get(k)
                if lw is not None:
                    deps.add(lw)
                self.readers.setdefault(k, []).append(i)
        for ap in w:
            for k in self.pages(ap):
                lw = self.lastw.get(k)
                if lw is not None:
                    deps.add(lw)
                rs = self.readers.get(k)
                if rs:
                    deps.update(rs)
                    self.readers[k] = []
                self.lastw[k] = i
        deps.discard(i)
        if dma:
            q = eng
            n = self.dma_count[q]
            self.dma_count[q] = n + 1
            hist = self.dma_hist[q]
            P = self.POOLSZ[q]
            o.sem = (q, n % P)
            o.target = 16 * (n // P + 1)
            if n >= P:
                o.extra_wait = hist[n - P]
            hist.append(i)
        self.ops.append(o)
        return o

    def emit(self):
        nc = self.nc
        ops = self.ops
        for o in ops:
            keep = set()
            for d in o.deps:
                p = ops[d]
                if (not p.dma) and (not o.dma) and p.eng == o.eng and (o.eng == "pe" or not (o.strict or STRICT_ALL)):
                    continue
                keep.add(d)
            if o.extra_wait is not None:
                keep.add(o.extra_wait)
            o.deps = keep
            for d in keep:
                if not ops[d].dma:
                    ops[d].sig = True
        cnt = {e: 0 for e in self.ENGS}
        sigidx = {}
        for o in ops:
            if o.sig and not o.dma:
                cnt[o.eng] += 1
                sigidx[o.idx] = cnt[o.eng]
        from contextlib import ExitStack
        with ExitStack() as es:
            esem = {e: es.enter_context(nc.semaphore("s_" + e)) for e in self.ENGS}
            dsem = {}
            for q, P in self.POOLSZ.items():
                for j in range(P):
                    dsem[(q, j)] = es.enter_context(nc.semaphore(f"d_{q}{j}"))
            block = es.enter_context(nc.Block())
            per = {e: [o for o in ops if o.eng == e] for e in self.ENGS}
            final = {}
            for o in ops:
                if o.dma:
                    final[o.sem] = max(final.get(o.sem, 0), o.target)

            def run(ename, eng):
                waited = {}
                for o in per[ename]:
                    need = {}
                    for d in o.deps:
                        p = ops[d]
                        if p.dma:
                            s = dsem[p.sem]
                            v = p.target
                        else:
                            s = esem[p.eng]
                            v = sigidx[p.idx]
                        key = id(s)
                        if need.get(key, (None, 0))[1] < v:
                            need[key] = (s, v)
                    for key, (s, v) in need.items():
                        if waited.get(key, 0) >= v:
                            continue
                        eng.wait_ge(s, v)
                        waited[key] = v
                    ins = o.fn(eng)
                    if o.dma:
                        ins.then_inc(dsem[o.sem], 16)
                    elif o.sig:
                        ins.then_inc(esem[ename], 1)
                if ename == "sp":
                    for k, v in final.items():
                        eng.wait_ge(dsem[k], v)

            @block.tensor
            def _(e):
                run("pe", e)

            @block.scalar
            def _(e):
                run("act", e)

            @block.vector
            def _(e):
                run("dve", e)

            @block.gpsimd
            def _(e):
                run("pool", e)

            @block.sync
            def _(e):
                run("sp", e)

    def mm(self, out, lhsT, rhs, start=True, stop=True):
        return self.op("pe", lambda e: e.matmul(out, lhsT, rhs, start=start, stop=stop), r=(lhsT, rhs), w=(out,))

    def transpose(self, out, in_, ident):
        return self.op("pe", lambda e: e.transpose(out, in_, ident), r=(in_, ident), w=(out,))

    def act(self, out, in_, func, bias=0.0, scale=1.0, accum_out=None, eng="act"):
        r = [in_]
        if not isinstance(bias, (int, float)):
            r.append(bias)
        if not isinstance(scale, (int, float)):
            r.append(scale)
        w = [out]
        kw = {}
        if accum_out is not None:
            w.append(accum_out)
            kw["accum_out"] = accum_out
        return self.op("act", lambda e: e.activation(out, in_, func, bias=bias, scale=scale, **kw), r=r, w=w)

    def tt(self, out, in0, in1, op, eng="dve"):
        return self.op(eng, lambda e: e.tensor_tensor(out, in0, in1, op), r=(in0, in1), w=(out,))

    def ts(self, out, in0, s1, s2=None, op0=ALU.mult, op1=None, eng="dve"):
        r = [in0]
        if not isinstance(s1, (int, float)):
            r.append(s1)
        if s2 is not None and not isinstance(s2, (int, float)):
            r.append(s2)
        if op1 is None:
            return self.op(eng, lambda e: e.tensor_scalar(out, in0, s1, None, op0), r=r, w=(out,))
        return self.op(eng, lambda e: e.tensor_scalar(out, in0, s1, s2, op0, op1), r=r, w=(out,))

    def stt(self, out, in0, scalar, in1, op0, op1):
        r = [in0, in1]
        if not isinstance(scalar, (int, float)):
            r.append(scalar)
        return self.op("dve", lambda e: e.scalar_tensor_tensor(out, in0, scalar, in1, op0, op1), r=r, w=(out,))

    def copy(self, out, in_, eng="dve"):
        if eng == "act":
            return self.op("act", lambda e: e.copy(out, in_), r=(in_,), w=(out,))
        return self.op(eng, lambda e: e.tensor_copy(out, in_), r=(in_,), w=(out,))

    def recip(self, out, in_):
        return self.op("dve", lambda e: e.reciprocal(out, in_), r=(in_,), w=(out,))

    def memset(self, out, val, eng="dve"):
        return self.op(eng, lambda e: e.memset(out, val), r=(), w=(out,))

    def dma(self, q, out, in_, **kw):
        return self.op(q, lambda e: e.dma_start(out=out, in_=in_, **kw), r=(in_,), w=(out,), dma=True)
import os
import numpy as np
import ml_dtypes
from concourse.bass_utils import run_bass_kernel_spmd

D = 1024
T = 2304
NCTX = 256
NLAT = 2048
DEPTH = 4
ALPHA = (2 * DEPTH) ** 0.25
EPS = 1e-6
TILES = [(0, 256), (256, 512), (768, 512), (1280, 512), (1792, 512)]
QCH = [(0, 3), (1, 4), (2, 5), (6, 9), (7, 10), (8, 11)]
NWIN = 2560


def host_prep(inp):
    f32 = np.float32
    sh = {}
    sh["w_mod"] = np.ascontiguousarray(inp["w_mod"], dtype=f32)
    sh["b_mod"] = np.ascontiguousarray(inp["b_mod"].reshape(4, 48, 128).transpose(2, 0, 1).reshape(128, 4 * 48))
    sh["ln_g"] = np.ascontiguousarray(inp["ln_g"].reshape(4, 2, 8, 128).transpose(3, 0, 1, 2).reshape(128, 64))
    sh["ln_b"] = np.ascontiguousarray(inp["ln_b"].reshape(4, 2, 8, 128).transpose(3, 0, 1, 2).reshape(128, 64))
    partner = np.arange(64) ^ 1
    cols = []
    def head_cols(base, h, part):
        idx = base + h * 64 + (partner if part else np.arange(64))
        return idx
    for (ha, hb) in QCH:
        cols.append(np.concatenate([head_cols(0, ha, False), head_cols(0, hb, False)]))
        cols.append(np.concatenate([head_cols(0, ha, True), head_cols(0, hb, True)]))
    for (ga, gb) in [(0, 1), (2, 3)]:
        cols.append(np.concatenate([head_cols(768, ga, False), head_cols(768, gb, False)]))
        cols.append(np.concatenate([head_cols(768, ga, True), head_cols(768, gb, True)]))
    cols.append(np.arange(1280, 1536))
    cols.append(np.arange(1024, 1280))
    cols = np.concatenate(cols)
    assert cols.shape[0] == NWIN
    sh["w_in_ab"] = np.ascontiguousarray(inp["w_in_ab"][:, :, cols])
    rows = np.concatenate([np.concatenate([np.arange(ha * 64, ha * 64 + 64), np.arange(hb * 64, hb * 64 + 64)]) for ha, hb in QCH] + [np.arange(768, 1024)])
    sh["w_out_ab"] = np.ascontiguousarray(inp["w_out_ab"][:, rows, :])
    g = np.zeros((128, 2, 4), f32)
    for i in range(2):
        g[:, i, 0] = np.tile(inp["q_gain"][i], 2)
        g[:, i, 1] = np.tile(inp["q_gain"][i][partner], 2)
        g[:, i, 2] = np.tile(inp["k_gain"][i], 2)
        g[:, i, 3] = np.tile(inp["k_gain"][i][partner], 2)
    sh["gains"] = g.reshape(128, 8)
    rows_ = NLAT // 64
    row = np.repeat(np.arange(rows_, dtype=f32), 64)
    col = np.tile(np.arange(64, dtype=f32), rows_)
    inv_freq = (1.0 / (10000.0 ** (np.arange(0, 32, 2, dtype=f32) / 32))).astype(f32)
    ang = np.concatenate([row[:, None] * inv_freq, col[:, None] * inv_freq], axis=-1).astype(f32)
    cos = np.cos(ang).astype(f32)
    sin = np.sin(ang).astype(f32)
    d = np.arange(128) % 64
    cosT = cos[:, d // 2].T
    sgn = np.where(d % 2 == 0, -1.0, 1.0).astype(f32)
    sinT = sin[:, d // 2].T * sgn[:, None]
    sh["rope"] = np.ascontiguousarray(np.stack([cosT, sinT], axis=1).astype(f32))
    def dft(n, scale):
        k = np.arange(n, dtype=np.int64)
        m = (k[:, None] * k[None, :]) % n
        a = 2.0 * np.pi * m.astype(np.float64) / n
        return (np.cos(a) * scale), (np.sin(a) * scale)
    c2048, s2048 = dft(2048, 1.0 / np.sqrt(2048.0))
    sh["dftc"] = c2048.astype(ml_dtypes.bfloat16)
    sh["dfts"] = s2048.astype(ml_dtypes.bfloat16)
    c256, s256 = dft(256, 1.0 / 16.0)
    sh["dftc256"] = c256.astype(ml_dtypes.bfloat16)
    sh["dfts256"] = s256.astype(ml_dtypes.bfloat16)
    c64, s64 = dft(64, 1.0 / 8.0)
    bd = np.zeros((128, 2, 128), f32)
    bd[0:64, 0, 0:64] = c64
    bd[64:128, 0, 64:128] = c64
    bd[0:64, 1, 0:64] = -s64
    bd[64:128, 1, 64:128] = -s64
    sh["dft64"] = bd.reshape(128, 256).astype(ml_dtypes.bfloat16)
    wf = np.zeros((2, 128, 2, 128), f32)
    for i in range(2):
        for gg in range(4):
            ch, o = gg // 2, (gg % 2) * 64
            wf[i, o:o + 64, ch, o:o + 64] = inp["w_fourier"][i, gg]
    sh["w_fourier"] = wf.reshape(2, 128, 256)
    sh["b_fourier"] = np.ascontiguousarray(inp["b_fourier"].reshape(2, 2, 128).transpose(2, 0, 1).reshape(128, 4))
    ccols = [np.arange(0, 1024)]
    for c in range(4):
        ccols.append(np.arange(1024 + c * 128, 1024 + (c + 1) * 128))
        ccols.append(np.arange(1536 + c * 128, 1536 + (c + 1) * 128))
    sh["w_in_cd"] = np.ascontiguousarray(inp["w_in_cd"][:, :, np.concatenate(ccols)])
    sh["w_out_cd"] = np.ascontiguousarray(inp["w_out_cd"], dtype=f32)
    sh["sgu_gb"] = np.ascontiguousarray(np.stack([inp["sgu_g"].reshape(2, 512), inp["sgu_b"].reshape(2, 512)], axis=1))
    sh["w_spT"] = np.ascontiguousarray(inp["w_spatial"].transpose(0, 3, 1, 2).reshape(2, 128, 512))
    sh["b_sp"] = np.ascontiguousarray(inp["b_spatial"].reshape(2, 512))
    sh["conv_w"] = np.ascontiguousarray(inp["conv_w"].reshape(2, 31, 4, 128).transpose(3, 0, 2, 1).reshape(128, 2 * 4 * 31))
    cv = np.stack([inp["conv_b"].reshape(2, 4, 128), inp["conv_norm_g"], inp["conv_norm_b"]], axis=2)
    sh["conv_v"] = np.ascontiguousarray(cv.transpose(3, 0, 1, 2).reshape(128, 24))
    wr = np.concatenate([inp["w_group"], inp["w_router"].transpose(0, 2, 1, 3).reshape(4, 1024, 32)], axis=2)
    sh["w_route"] = np.ascontiguousarray(wr)
    sh["b_route"] = np.ascontiguousarray(np.concatenate([inp["b_group"], inp["b_router"].reshape(4, 32)], axis=1))
    sh["w_exp_gate"] = inp["w_exp_gate"]
    sh["w_exp_up"] = inp["w_exp_up"]
    sh["w_exp_down"] = inp["w_exp_down"]
    per_core = []
    for i in range(8):
        m = dict(sh)
        b0 = 2 * i
        xt = np.empty((2, 1024, T), f32)
        for s in range(2):
            xt[s, :, :NCTX] = inp["ctx"][b0 + s].T
            xt[s, :, NCTX:] = inp["x"][b0 + s].T
        m["xT"] = xt
        c3 = np.stack([inp["c"][b0], inp["c"][b0 + 1], inp["c_ctx"]], axis=1)
        m["c3"] = np.ascontiguousarray(c3.reshape(8, 128, 3).transpose(1, 0, 2).reshape(128, 24))
        per_core.append(m)
    return per_core


class StopBuild(Exception):
    pass


class Prog:
    def __init__(self, layers=(0, 1, 2, 3), seqs=(0, 1), dbg=None, stop=None):
        self.layers = layers
        self.seqs = seqs
        self.dbg = dbg
        self.stop = stop
        nc = bass.Bass("TRN2", target_bir_lowering=False)
        self.nc = nc
        self.K = Sched(nc)
        self.declare_dram()
        self.alloc()
        self.prologue()
        for s in seqs:
            try:
                self.sequence(s)
            except StopBuild:
                pass
        self.K.emit()

    def declare_dram(self):
        nc = self.nc
        def din(name, shape, dt=F32):
            return nc.dram_tensor(name, list(shape), dt, kind="ExternalInput").ap()
        self.xT = din("xT", [2, 1024, T])
        self.c3 = din("c3", [128, 24])
        self.w_mod = din("w_mod", [4, 1024, 6144])
        self.b_mod = din("b_mod", [128, 192])
        self.ln_g = din("ln_g", [128, 64])
        self.ln_b = din("ln_b", [128, 64])
        self.w_in_ab = din("w_in_ab", [2, 1024, NWIN])
        self.w_out_ab = din("w_out_ab", [2, 1024, 1024])
        self.gains = din("gains", [128, 8])
        self.rope = din("rope", [128, 2, 2048])
        self.dftc = din("dftc", [2048, 2048], BF16)
        self.dfts = din("dfts", [2048, 2048], BF16)
        self.dftc256 = din("dftc256", [256, 256], BF16)
        self.dfts256 = din("dfts256", [256, 256], BF16)
        self.dft64 = din("dft64", [128, 256], BF16)
        self.w_fourier = din("w_fourier", [2, 128, 256])
        self.b_fourier = din("b_fourier", [128, 4])
        self.w_in_cd = din("w_in_cd", [2, 1024, 2048])
        self.w_out_cd = din("w_out_cd", [2, 1024, 1024])
        self.sgu_gb = din("sgu_gb", [2, 2, 512])
        self.w_spT = din("w_spT", [2, 128, 512])
        self.b_sp = din("b_sp", [2, 512])
        self.conv_w = din("conv_w", [128, 248])
        self.conv_v = din("conv_v", [128, 24])
        self.w_route = din("w_route", [4, 1024, 36])
        self.b_route = din("b_route", [4, 36])
        self.w_exp_gate = din("w_exp_gate", [4, 32, 1024, 256])
        self.w_exp_up = din("w_exp_up", [4, 32, 1024, 256])
        self.w_exp_down = din("w_exp_down", [4, 32, 256, 1024])
        self.outT = nc.dram_tensor("outT", [2, 1024, NLAT], F32, kind="ExternalOutput").ap()
        if self.dbg:
            self.dbgT = nc.dram_tensor("dbgT", [1024, T], F32, kind="ExternalOutput").ap()

    def alloc(self):
        K = self.K
        o = ARENA0
        def take(n):
            nonlocal o
            r = o
            o += (n + 31) // 32 * 32
            return r
        self.X = K.sb("X", [128, 8, T], F32, take(8 * T * 4))
        self.MOD = K.sb("MOD", [128, 4, 48, 3], F32, take(4 * 48 * 3 * 4))
        self.BMOD = K.sb("BMOD", [128, 4, 48], F32, take(192 * 4))
        self.LNG = K.sb("LNG", [128, 4, 2, 8], F32, take(256))
        self.LNB = K.sb("LNB", [128, 4, 2, 8], F32, take(256))
        self.GAINS = K.sb("GAINS", [128, 2, 4], F32, take(32))
        self.BF = K.sb("BFc", [128, 2, 2], F32, take(16))
        self.CONVW = K.sb("CONVW", [128, 2, 4, 31], F32, take(248 * 4))
        self.CONVV = K.sb("CONVV", [128, 2, 4, 3], F32, take(96))
        self.C3 = K.sb("C3", [128, 8, 3], F32, take(96))
        self.S3 = K.sb("S3", [128, 8, 3], BF16, take(48))
        self.IDB = K.sb("IDB", [128, 128], BF16, take(256))
        self.IDF = K.sb("IDF", [128, 128], F32, take(512))
        self.ONESF = K.sb("ONESF", [128, 128], F32, take(512))
        self.ONESG = K.sb("ONESG", [128, 128], F32, take(512))
        self.BONES = K.sb("BONES", [128, 128], BF16, take(256))
        self.SEL = K.sb("SEL", [64, 32, 128], BF16, take(32 * 128 * 2))
        self.EPSC = K.sb("EPSC", [128, 4], F32, take(16))
        self.DFT64 = K.sb("DFT64", [128, 2, 128], BF16, take(512))
        self.BRT = K.sb("BRT", [128, 4, 36], F32, take(4 * 36 * 4))
        o = (o + 255) // 256 * 256
        self.SC0 = o
        self.SCN = ARENA1 - o
        S = self.SC0
        self.AT = K.sb("AT", [128, 8, T], BF16, S + 0)
        self.KT = K.sb("KT", [128, 2, T], BF16, S + 36864)
        self.V = K.sb("V", [128, 18, 4, 128], BF16, S + 46080)
        self.HT = [K.sb("HT0", [128, 8, 512], BF16, S + 64512)] * 2
        self.WB = [K.sb(f"WB{i}", [128, 8, 512], BF16, S + 80896 + i * 8192) for i in range(2)]
        self.ROPE = [K.sb(f"ROPE{i}", [128, 2, 512], F32, S + 97280 + i * 4096) for i in range(2)]
        self.ABT = []
        for i_ in range(2):
            b = (S + 105472) if i_ == 0 else (S + 72704)
            b2 = (S + 105472 + 7168) if i_ == 0 else (S + 116736)
            d_ = {}
            d_["SQb"] = K.sb(f"SQb{i_}", [128, 512], BF16, b)
            d_["QG"] = K.sb(f"QG{i_}", [128, 512], F32, b + 1024)
            d_["Q2G"] = K.sb(f"Q2G{i_}", [128, 512], F32, b + 3072)
            d_["SD"] = K.sb(f"SD{i_}", [128, 512], F32, b + 5120)
            d_["T1"] = K.sb(f"T1{i_}", [128, 512], F32, b2)
            d_["T2"] = K.sb(f"T2{i_}", [128, 512], F32, b2 + 2048)
            self.ABT.append(d_)
        self.PT = [K.sb(f"PT{i}", [128, 512], BF16, S + 64512 + i * 1024) for i in range(6)]
        self.RC = [K.sb(f"RC{i}", [64, 512], F32, S + 64512 + 6144 + i * 2048) for i in range(2)]
        self.UV = K.sb("UV", [128, 18, 512], BF16, S + 36864)
        self.DC = K.sb("DC", [128, 16, 512], BF16, S + 55296)
        self.DS = K.sb("DS", [128, 16, 512], BF16, S + 71680)
        self.MX = K.sb("MX", [128, 2, 512], BF16, S + 88064)
        self.WFB = K.sb("WFB", [128, 2, 128], BF16, S + 90112)
        self.VG = K.sb("VG", [128, 18, 512], BF16, S + 36864)
        self.GLU = K.sb("GLU", [128, 4, 2364], BF16, S + 55296)
        self.CHT = [K.sb(f"CHT{i}", [128, 8, 512], BF16, S + 74240 + i * 8192) for i in range(2)]
        self.CWB = [K.sb(f"CWB{i}", [128, 8, 512], BF16, S + 90624 + i * 8192) for i in range(2)]
        self.DG = K.sb("DG", [128, 4, 31, 128], BF16, S + 74240)
        b = S + 107008
        self.CVV = K.sb("CVV", [128, 512], F32, b)
        self.CSG = K.sb("CSG", [128, 512], F32, b + 2048)
        self.GBC = K.sb("GBC", [128, 2, 512], F32, b + 4096)
        self.BST = K.sb("BST", [128, 4, 6], F32, b + 8192)
        self.MV = K.sb("MV", [128, 4, 2], F32, b + 8192 + 96)
        self.RS4 = K.sb("RS4", [128, 4], F32, b + 8192 + 128)
        self.SVt = K.sb("SVt", [128, 512], F32, b + 8448)
        self.WSP = K.sb("WSP", [128, 4, 128], BF16, b + 10496)
        self.BSPb = K.sb("BSPb", [128, 4, 128], F32, b + 11520)
        assert b + 13568 <= S + self.SCN
        self.CM = K.sb("CM", [128, 512], F32, S + 36864)
        self.CV2 = [dict(dw=K.sb(f"cdw{i}", [128, 512], F32, S + 38912 + i * 8192), dsq=K.sb(f"cdsq{i}", [128, 512], F32, S + 40960 + i * 8192),
                         M=K.sb(f"cM{i}", [128, 512], F32, S + 43008 + i * 8192), VV=K.sb(f"cVV{i}", [128, 512], F32, S + 45056 + i * 8192)) for i in range(2)]
        self.WO = K.sb("WO", [128, 8, 1024], BF16, S + 36864)
        b = S + 53248
        self.LSQ = [K.sb(f"LSQ{i}", [128, 512], F32, b + i * 2048) for i in range(2)]
        self.LM = K.sb("LM", [128, 512], F32, b + 4096)
        self.LMS = K.sb("LMS", [128, 512], F32, b + 6144)
        self.LV = K.sb("LV", [128, 512], F32, b + 8192)
        self.LZ = [K.sb(f"LZ{i}", [128, 512], F32, b + 10240 + i * 2048) for i in range(2)]
        self.LM2 = K.sb("LM2", [128, 512], F32, b + 14336)
        self.LMS2 = K.sb("LMS2", [128, 512], F32, b + 16384)
        self.LV2 = K.sb("LV2", [128, 512], F32, b + 18432)
        self.ln_par = 0
        self.H = K.sb("H", [128, 8, T], BF16, S + self.SCN - 36864 - 64)
        self.WG = [K.sb(f"WG{i}", [128, 2, 8, 256], BF16, S + i * 24576) for i in range(2)]
        self.WU = [K.sb(f"WU{i}", [128, 2, 8, 256], BF16, S + i * 24576 + 8192) for i in range(2)]
        self.WD = [K.sb(f"WD{i}", [128, 2, 2, 1024], BF16, S + i * 24576 + 16384) for i in range(2)]
        self.COMBT = K.sb("COMBT", [64, T], BF16, S + 49152)
        self.HID = [K.sb(f"HID{i}", [128, 4, 512], BF16, S + 53760 + i * 4096) for i in range(2)]
        self.SG = [K.sb(f"SG{i}", [128, 512], F32, S + 61952 + i * 2048) for i in range(2)]
        self.TM = [K.sb(f"TM{i}", [128, 512], F32, S + 66048 + i * 2048) for i in range(2)]
        self.HF = K.sb("HF", [128, 8, 128], F32, S + 70144)
        self.WR = K.sb("WR", [128, 8, 36], F32, S + 74240)
        b = S + 75392
        self.RL = K.sb("RL", [128, 36], F32, b)
        self.RT = K.sb("RT", [128, 64], F32, b + 160)
        self.RCMB = K.sb("RCMB", [128, 32], F32, b + 416)
        self.RCB2 = K.sb("RCB2", [128, 64], BF16, b + 544)
        self.RTOP = K.sb("RTOP", [128, 8], F32, b + 672)
        assert S + self.SCN - 36864 - 64 >= b + 1024
        print("scratch bytes", self.SCN, "start", self.SC0)
        self.PS = [K.ps(f"ps{i}") for i in range(8)]

    def dbg_dump(self, src, ncols=T, chunks=8):
        K = self.K
        for c in range(chunks):
            K.dma("pool", self.dbgT[c * 128:(c + 1) * 128, 0:ncols], src[:, c, 0:ncols])

    def prologue(self):
        K = self.K
        nc = self.nc
        K.dma("sp", self.C3[:].rearrange("p c n -> p (c n)"), self.c3[:, :])
        K.dma("sp", self.BMOD[:].rearrange("p l j -> p (l j)"), self.b_mod[:, :])
        K.dma("sp", self.LNG[:].rearrange("p l s c -> p (l s c)"), self.ln_g[:, :])
        K.dma("sp", self.LNB[:].rearrange("p l s c -> p (l s c)"), self.ln_b[:, :])
        K.dma("sp", self.GAINS[:].rearrange("p i k -> p (i k)"), self.gains[:, :])
        K.dma("sp", self.BF[:].rearrange("p i k -> p (i k)"), self.b_fourier[:, :])
        K.dma("sp", self.CONVW[:].rearrange("p i c k -> p (i c k)"), self.conv_w[:, :])
        K.dma("sp", self.CONVV[:].rearrange("p i c k -> p (i c k)"), self.conv_v[:, :])
        K.dma("sp", self.DFT64[:].rearrange("p a b -> p (a b)"), self.dft64[:, :])
        for l in range(4):
            K.dma("sp", self.BRT[:, l, :], self.b_route[l:l + 1, :].partition_broadcast(128))
        K.strict = True
        K.memset(self.ONESF[:], 1.0 / 1024.0)
        K.memset(self.ONESG[:], 1.0 / 128.0)
        K.memset(self.EPSC[:, 0:1], EPS / (ALPHA * ALPHA))
        K.memset(self.EPSC[:, 1:2], EPS)
        K.memset(self.EPSC[:, 2:3], 1.0)
        K.memset(self.BONES[:], 0.0)
        K.memset(self.BONES[0:64, 0:64], 1.0 / 64.0)
        K.memset(self.BONES[64:128, 64:128], 1.0 / 64.0)
        K.memset(self.IDF[:], 0.0, eng="pool")
        K.op("pool", lambda e: e.affine_select(self.IDF[:], self.IDF[:], [[-1, 128]], ALU.not_equal, 1.0, base=0, channel_multiplier=1),
             r=(self.IDF[:],), w=(self.IDF[:],))
        K.copy(self.IDB[:], self.IDF[:])
        K.memset(self.SEL[:], 0.0)
        for e in range(32):
            K.copy(self.SEL[0:64, e, :], self.IDB[0:64, e:e + 1].to_broadcast([64, 128]))
            K.tt(self.SEL[0:64, e, :], self.SEL[0:64, e, :], self.IDB[0:64, 32 + e:33 + e].to_broadcast([64, 128]), ALU.add)
        K.act(self.S3[:], self.C3[:], AF.Silu)
        K.strict = False
        WB = [K.sb(f"WMB{i}", [128, 8, 1024], BF16, self.SC0 + i * 16384) for i in range(2)]
        it = 0
        for l in self.layers:
            pm = self.PS[0]
            for blk in range(6):
                wb = WB[it % 2]
                it += 1
                K.dma("pool", wb[:], self.w_mod[l, :, blk * 1024:(blk + 1) * 1024].rearrange("(c p) n -> p c n", p=128))
                for jj in range(8):
                    j = blk * 8 + jj
                    for k in range(8):
                        K.mm(pm[:, j * 3:(j + 1) * 3], wb[:, k, jj * 128:(jj + 1) * 128], self.S3[:, k, :], start=(k == 0), stop=(k == 7))
            K.strict = True
            K.tt(self.MOD[:, l, :, :], pm[:, 0:144].rearrange("p (j n) -> p j n", n=3),
                 self.BMOD[:, l, :].unsqueeze(2).to_broadcast([128, 48, 3]), ALU.add)
            K.ts(self.MOD[:, l, 8:16, :], self.MOD[:, l, 8:16, :], 1.0, None, ALU.add)
            K.ts(self.MOD[:, l, 32:40, :], self.MOD[:, l, 32:40, :], 1.0, None, ALU.add)
            K.ts(self.MOD[:, l, 16:24, :], self.MOD[:, l, 16:24, :], 1.0 / ALPHA, None, ALU.mult)
            K.ts(self.MOD[:, l, 40:48, :], self.MOD[:, l, 40:48, :], 1.0 / ALPHA, None, ALU.mult)
            K.strict = False

    def mod(self, l, part, c, n):
        return self.MOD[:, l, part * 8 + c, n:n + 1]

    def sequence(self, s):
        K = self.K
        for (t0, sz) in TILES:
            K.dma("sp", self.X[:, :, t0:t0 + sz], self.xT[s, :, t0:t0 + sz].rearrange("(c p) t -> p c t", p=128))
        for l in self.layers:
            ctx_out = l < 2
            ctx_kv = (l == 2)
            if l % 2 == 0:
                self.mixer_ab(s, l, ctx_out)
            else:
                self.mixer_cd(s, l, ctx_out)
            if self.stop == ("mix", l):
                self.dbg_dump(self.AT)
                return
            self.wout_ln1(s, l, ctx_out)
            if self.stop == ("ln1", l):
                self.dbg_dump(self.X)
                return
            self.moe(s, l, ctx_out)
            if self.stop == ("moe", l):
                self.dbg_dump(self.X)
                return
        for c in range(8):
            K.dma("sp", self.outT[s, c * 128:(c + 1) * 128, :], self.X[:, c, NCTX:T])

    def ln_tile(self, l, st, t0, sz, epscol):
        K = self.K
        X = self.X
        par = self.ln_par
        self.ln_par = 1 - par
        s1, s2 = self.PS[6 - 2 * par], self.PS[7 - 2 * par]
        for c in range(8):
            sq = self.LSQ[c % 2]
            K.act(sq[:, :sz], X[:, c, t0:t0 + sz], AF.Square)
            K.mm(s1[:, :sz], self.ONESF[:], X[:, c, t0:t0 + sz], start=(c == 0), stop=(c == 7))
            K.mm(s2[:, :sz], self.ONESF[:], sq[:, :sz], start=(c == 0), stop=(c == 7))
        import os
        LNCUT = int(os.environ.get("LNCUT", "9"))
        if LNCUT < 2:
            return
        M, MS, VV = (self.LM, self.LMS, self.LV) if par == 0 else (self.LM2, self.LMS2, self.LV2)
        K.act(MS[:, :sz], s1[:, :sz], AF.Square)
        K.tt(VV[:, :sz], s2[:, :sz], MS[:, :sz], ALU.subtract)
        K.act(VV[:, :sz], VV[:, :sz], AF.Ln, bias=self.EPSC[:, epscol:epscol + 1])
        K.act(s2[:, :sz], VV[:, :sz], AF.Exp, scale=-0.5)
        for c in range(8):
            z = self.LZ[c % 2]
            K.tt(z[:, :sz], X[:, c, t0:t0 + sz], s1[:, :sz], ALU.subtract)
            K.tt(z[:, :sz], z[:, :sz], s2[:, :sz], ALU.mult)
            K.act(X[:, c, t0:t0 + sz], z[:, :sz], AF.Identity, bias=self.LNB[:, l, st, c:c + 1], scale=self.LNG[:, l, st, c:c + 1])

    def wout_ln1(self, s, l, ctx_out):
        K = self.K
        X, AT, WO, H = self.X, self.AT, self.WO, self.H
        i = l // 2
        wsrc = self.w_out_ab if l % 2 == 0 else self.w_out_cd
        K.dma("pool", WO[:], wsrc[i].rearrange("(c p) n -> p c n", p=128))
        for ti, (t0, sz) in enumerate(TILES):
            if ti == 0 and not ctx_out:
                continue
            n = 2 if ti == 0 else s
            for oc in range(8):
                ps = self.PS[oc % 4]
                for k in range(8):
                    K.mm(ps[:, :sz], WO[:, k, oc * 128:(oc + 1) * 128], AT[:, k, t0:t0 + sz], start=(k == 0), stop=(k == 7))
                K.stt(X[:, oc, t0:t0 + sz], ps[:, :sz], self.mod(l, 2, oc, n), X[:, oc, t0:t0 + sz], ALU.mult, ALU.add)
            import os
            if os.environ.get("NOLN"):
                continue
            self.ln_tile(l, 0, t0, sz, 0)
            if os.environ.get("NOH"):
                continue
            for c in range(8):
                if True:
                    K.ts(H[:, c, t0:t0 + sz], X[:, c, t0:t0 + sz], self.mod(l, 4, c, n), self.mod(l, 3, c, n), ALU.mult, ALU.add)
                else:
                    K.act(H[:, c, t0:t0 + sz], X[:, c, t0:t0 + sz], AF.Identity, bias=self.mod(l, 3, c, n), scale=self.mod(l, 4, c, n))

    def mixer_ab(self, s, l, ctx_out):
        K = self.K
        X, AT, KT, V = self.X, self.AT, self.KT, self.V
        PS = self.PS
        i = l // 2
        G = self.GAINS
        K.memset(V[:, :, :, 64:128], 1.0)
        wit = 0
        for ti, (t0, sz) in enumerate(TILES):
            is_ctx = (ti == 0)
            n = 2 if is_ctx else s
            ht = self.HT[ti % 2]
            for c in range(8):
                K.act(ht[:, c, :sz], X[:, c, t0:t0 + sz], AF.Identity, bias=self.mod(l, 0, c, n), scale=self.mod(l, 1, c, n))
            rp = self.ROPE[ti % 2]
            if not is_ctx:
                K.dma("sp", rp[:, :, :sz], self.rope[:, :, t0 - NCTX:t0 - NCTX + sz])
            import os
            CUT = int(os.environ.get("CUT", "99"))
            for blk in range(5):
                if is_ctx and blk < 3 and not ctx_out:
                    continue
                if blk >= CUT:
                    continue
                wb = self.WB[wit % 2]
                wit += 1
                K.dma("pool", wb[:], self.w_in_ab[i, :, blk * 512:(blk + 1) * 512].rearrange("(c p) n -> p c n", p=128))

                def proj(ps, col0, ncol=128):
                    for k in range(8):
                        K.mm(ps[0:ncol, :sz], wb[:, k, col0:col0 + ncol], ht[:, k, :sz], start=(k == 0), stop=(k == 7))

                if blk < 4:
                    for cc in range(2):
                        if blk < 3:
                            j = blk * 2 + cc
                            dest = AT[:, j, t0:t0 + sz]
                            g0 = 0
                        else:
                            j = cc
                            dest = KT[:, j, t0:t0 + sz]
                            g0 = 2
                        pq, pq2, ss = PS[cc], PS[2 + cc], PS[4 + cc]
                        tset = self.ABT[cc]
                        SQb, QG, Q2G, SD, T1, T2 = tset["SQb"], tset["QG"], tset["Q2G"], tset["SD"], tset["T1"], tset["T2"]
                        proj(pq, cc * 256)
                        if not is_ctx:
                            proj(pq2, cc * 256 + 128)
                        K.act(SQb[:, :sz], pq[:, :sz], AF.Square)
                        K.act(QG[:, :sz], pq[:, :sz], AF.Identity, scale=G[:, i, g0:g0 + 1])
                        K.mm(ss[:, :sz], self.BONES[:], SQb[:, :sz])
                        self.rstd(SD[:, :sz], ss[:, :sz], 1)
                        if not is_ctx:
                            K.act(Q2G[:, :sz], pq2[:, :sz], AF.Identity, scale=G[:, i, g0 + 1:g0 + 2])
                            K.tt(T1[:, :sz], QG[:, :sz], rp[:, 0, :sz], ALU.mult)
                            K.tt(T2[:, :sz], Q2G[:, :sz], rp[:, 1, :sz], ALU.mult, eng="pool")
                            K.tt(T1[:, :sz], T1[:, :sz], T2[:, :sz], ALU.add)
                            K.tt(dest, T1[:, :sz], SD[:, :sz], ALU.mult)
                        else:
                            K.tt(dest, QG[:, :sz], SD[:, :sz], ALU.mult)
                else:
                    if ((not is_ctx) or ctx_out) and os.environ.get("NOF") is None:
                        for fc in range(2):
                            ps = PS[fc]
                            proj(ps, fc * 128)
                            K.copy(AT[:, 6 + fc, t0:t0 + sz], ps[:, :sz], eng="act")
                    for up in range(sz // 256 if os.environ.get("NOV") is None else 0):
                        tc = (t0 + up * 256) // 128
                        ps = PS[6 + up % 2]
                        for uu in range(2):
                            u = up * 2 + uu
                            o = uu * 256
                            for k in range(8):
                                K.mm(ps[:, o:o + 256], ht[:, k, u * 128:(u + 1) * 128], wb[:, k, 256:512], start=(k == 0), stop=(k == 7))
                        K.copy(V[:, tc:tc + 2, :, 0:64], ps[:, 0:512].rearrange("p (u g d) -> p u g d", u=2, d=64))
        if self.stop == ("proj", l):
            self.dbg_dump(self.AT)
            raise StopBuild()
        calls = []
        if ctx_out:
            calls.append((0, 256, 2))
        for (t0, sz) in TILES[1:]:
            calls.append((t0, sz, 18))
        cn = 0
        for j in range(6):
            kc = j // 3
            gA, gB = 2 * kc, 2 * kc + 1
            for (t0, sz, nk) in calls:
                OA, OB = PS[4 + 2 * (cn % 2)], PS[5 + 2 * (cn % 2)]
                cn += 1
                pts = {}
                for kk in range(nk + 1):
                    if kk < nk:
                        SA, SB = PS[(kk % 2) * 2], PS[(kk % 2) * 2 + 1]
                        pa, pb = self.PT[(kk % 3) * 2], self.PT[(kk % 3) * 2 + 1]
                        K.mm(SA[:, :sz], KT[0:64, kc, kk * 128:(kk + 1) * 128], AT[0:64, j, t0:t0 + sz])
                        K.mm(SB[:, :sz], KT[64:128, kc, kk * 128:(kk + 1) * 128], AT[64:128, j, t0:t0 + sz])
                        K.act(pa[:, :sz], SA[:, :sz], AF.Exp, scale=0.125)
                        K.act(pb[:, :sz], SB[:, :sz], AF.Exp, scale=0.125)
                        pts[kk] = (pa, pb)
                    if kk >= 1:
                        pa, pb = pts.pop(kk - 1)
                        K.mm(OA[:, :sz], V[:, kk - 1, gA, :], pa[:, :sz], start=(kk == 1), stop=(kk == nk))
                        K.mm(OB[:, :sz], V[:, kk - 1, gB, :], pb[:, :sz], start=(kk == 1), stop=(kk == nk))
                K.recip(self.RC[0][0:64, :sz], OA[64:128, :sz])
                K.tt(AT[0:64, j, t0:t0 + sz], OA[0:64, :sz], self.RC[0][0:64, :sz], ALU.mult)
                K.recip(self.RC[1][0:64, :sz], OB[64:128, :sz])
                K.tt(AT[64:128, j, t0:t0 + sz], OB[0:64, :sz], self.RC[1][0:64, :sz], ALU.mult)
        if self.stop == ("attn", l):
            self.dbg_dump(self.AT)
            raise StopBuild()
        UV, DC, DS, MX, WFB = self.UV, self.DC, self.DS, self.MX, self.WFB
        K.dma("pool", WFB[:].rearrange("p a b -> p (a b)"), self.w_fourier[i, :, :])
        tcs = list(range(2, 18)) + ([0, 1] if ctx_out else [])
        for q, tc in enumerate(tcs):
            ps = PS[q % 2]
            for cs in range(2):
                for fc in range(2):
                    o = (cs * 2 + fc) * 128
                    K.mm(ps[:, o:o + 128], AT[:, 6 + fc, tc * 128:(tc + 1) * 128], self.DFT64[:, cs, :])
            K.copy(UV[:, tc, :], ps[:, :], eng=("act" if q % 2 else "dve"))
        jobs = [(256 + nt * 512, 512, 2, 16, nt) for nt in range(4)]
        if ctx_out:
            jobs.append((0, 256, 0, 2, -1))
        for (t0, sz, tc0, ntc, nt) in jobs:
            if nt >= 0:
                K.dma("sp", DC[:, :, :], self.dftc[:, nt * 512:(nt + 1) * 512].rearrange("(c p) n -> p c n", p=128))
                K.dma("sp", DS[:, :, :], self.dfts[:, nt * 512:(nt + 1) * 512].rearrange("(c p) n -> p c n", p=128))
            else:
                K.dma("sp", DC[:, 0:2, 0:256], self.dftc256[:, :].rearrange("(c p) n -> p c n", p=128))
                K.dma("sp", DS[:, 0:2, 0:256], self.dfts256[:, :].rearrange("(c p) n -> p c n", p=128))
            for fc in range(2):
                ps = PS[2 + fc]
                for a in range(ntc):
                    K.mm(ps[:, :sz], UV[:, tc0 + a, fc * 128:(fc + 1) * 128], DC[:, a, :sz], start=(a == 0), stop=False)
                for a in range(ntc):
                    K.mm(ps[:, :sz], UV[:, tc0 + a, 256 + fc * 128:256 + (fc + 1) * 128], DS[:, a, :sz], start=False, stop=(a == ntc - 1))
                K.copy(MX[:, fc, :sz], ps[:, :sz], eng="act")
                ps2 = PS[4 + fc]
                K.mm(ps2[:, :sz], WFB[:, fc, :], MX[:, fc, :sz])
                K.act(AT[:, 6 + fc, t0:t0 + sz], ps2[:, :sz], AF.Identity, bias=self.BF[:, i, fc:fc + 1])

    def rstd(self, out, in_, epscol):
        K = self.K
        K.act(out, in_, AF.Ln, bias=self.EPSC[:, epscol:epscol + 1])
        K.act(out, out, AF.Exp, scale=-0.5)

    def sigmoid(self, out, in_, scale=1.0):
        K = self.K
        K.act(out, in_, AF.Exp, scale=-scale)
        K.act(out, out, AF.Ln, bias=self.EPSC[:, 2:3])
        K.act(out, out, AF.Exp, scale=-1.0)

    def gelu(self, out, ps, tmp, tmp2):
        K = self.K
        K.act(tmp, ps, AF.Square)
        K.ts(tmp, tmp, 0.044715, 1.0, ALU.mult, ALU.add)
        K.tt(tmp, tmp, ps, ALU.mult)
        self.sigmoid(tmp2, tmp, 1.5957691216057308)
        K.tt(out, tmp2, ps, ALU.mult)

    def mixer_cd(self, s, l, ctx_out):
        K = self.K
        X, YT, VG, GLU = self.X, self.AT, self.VG, self.GLU
        PS = self.PS
        i = l // 2
        K.dma("sp", self.GBC[:, 0, :], self.sgu_gb[i, 0:1, :].partition_broadcast(128))
        K.dma("sp", self.GBC[:, 1, :], self.sgu_gb[i, 1:2, :].partition_broadcast(128))
        K.memset(GLU[:, :, 0:15], 0.0)
        K.memset(GLU[:, :, 271:301], 0.0)
        K.memset(GLU[:, :, 2349:2364], 0.0)
        def gcol(t0):
            return 15 + t0 if t0 < NCTX else 301 + (t0 - NCTX)
        wit = 0
        for ti, (t0, sz) in enumerate(TILES):
            is_ctx = (ti == 0)
            if is_ctx and not ctx_out:
                continue
            n = 2 if is_ctx else s
            ht = self.CHT[ti % 2]
            for c in range(8):
                K.act(ht[:, c, :sz], X[:, c, t0:t0 + sz], AF.Identity, bias=self.mod(l, 0, c, n), scale=self.mod(l, 1, c, n))
            for blk in range(4):
                wb = self.CWB[wit % 2]
                wit += 1
                K.dma("pool", wb[:], self.w_in_cd[i, :, blk * 512:(blk + 1) * 512].rearrange("(c p) n -> p c n", p=128))
                if blk == 0:
                    for c in range(4):
                        ps = PS[c % 2]
                        for k in range(8):
                            K.mm(ps[:, :sz], wb[:, k, c * 128:(c + 1) * 128], ht[:, k, :sz], start=(k == 0), stop=(k == 7))
                        self.gelu(YT[:, c, t0:t0 + sz], ps[:, :sz], self.CVV[:, :sz], self.CSG[:, :sz])
                elif blk == 1:
                    for u in range(sz // 128):
                        tc = (t0 + u * 128) // 128
                        ps = PS[2 + u % 2]
                        for k in range(8):
                            K.mm(ps[:, :], ht[:, k, u * 128:(u + 1) * 128], wb[:, k, :], start=(k == 0), stop=(k == 7))
                        vv = self.SVt
                        self.gelu(vv[:, :], ps[:, :], self.CVV[:, :], self.CSG[:, :])
                        K.strict = True
                        for g in range(4):
                            K.op("dve", lambda e, g=g: e.bn_stats(self.BST[:, g, :], vv[:, g * 128:(g + 1) * 128]),
                                 r=(vv[:, g * 128:(g + 1) * 128],), w=(self.BST[:, g, :],))
                            K.op("dve", lambda e, g=g: e.bn_aggr(self.MV[:, g, :], self.BST[:, g, :]),
                                 r=(self.BST[:, g, :],), w=(self.MV[:, g, :],))
                        self.rstd(self.RS4[:, :], self.MV[:, :, 1], 1)
                        for g in range(4):
                            K.ts(vv[:, g * 128:(g + 1) * 128], vv[:, g * 128:(g + 1) * 128], self.MV[:, g, 0:1], self.RS4[:, g:g + 1], ALU.subtract, ALU.mult)
                        K.strict = False
                        K.tt(vv[:, :], vv[:, :], self.GBC[:, 0, :], ALU.mult)
                        K.tt(VG[:, tc, :], vv[:, :], self.GBC[:, 1, :], ALU.add)
                else:
                    for cc in range(2):
                        c = (blk - 2) * 2 + cc
                        pa, pg = PS[4 + cc], PS[6 + cc]
                        for k in range(8):
                            K.mm(pa[:, :sz], wb[:, k, cc * 256:cc * 256 + 128], ht[:, k, :sz], start=(k == 0), stop=(k == 7))
                        for k in range(8):
                            K.mm(pg[:, :sz], wb[:, k, cc * 256 + 128:cc * 256 + 256], ht[:, k, :sz], start=(k == 0), stop=(k == 7))
                        self.sigmoid(self.CSG[:, :sz], pg[:, :sz])
                        gc = gcol(t0)
                        K.tt(GLU[:, c, gc:gc + sz], pa[:, :sz], self.CSG[:, :sz], ALU.mult)
        if self.stop == ("cdproj", l):
            for c in range(4):
                K.dma("pool", self.dbgT[c * 128:(c + 1) * 128, 0:T], self.VG[:].rearrange("p t f -> p (t f)")[:, c * T:(c + 1) * T])
            raise StopBuild()
        DG = self.DG
        for c in range(4):
            for k in range(31):
                K.ts(DG[:, c, k, :], self.IDB[:], self.CONVW[:, i, c, k:k + 1], None, ALU.mult, eng="dve")
        K.dma("pool", self.WSP[:].rearrange("p g q -> p (g q)"), self.w_spT[i, :, :])
        K.dma("sp", self.BSPb[:].rearrange("p g q -> p (g q)"), self.b_sp[i:i + 1, :].partition_broadcast(128))
        groups = ([[0, 1]] if ctx_out else []) + [[2 + 4 * a + b for b in range(4)] for a in range(4)]
        q = 0
        for g in range(4):
            for grp in groups:
                ps = PS[q % 2]
                q += 1
                na = len(grp)
                for a, tc in enumerate(grp):
                    K.mm(ps[:, a * 128:(a + 1) * 128], VG[:, tc, g * 128:(g + 1) * 128], self.WSP[:, g, :])
                c0 = grp[0] * 128
                K.tt(self.SVt[:, :na * 128].rearrange("p (a q) -> p a q", q=128), ps[:, :na * 128].rearrange("p (a q) -> p a q", q=128),
                     self.BSPb[:, g, :].unsqueeze(1).to_broadcast([128, na, 128]), ALU.add)
                K.tt(YT[:, g, c0:c0 + na * 128], self.SVt[:, :na * 128], YT[:, g, c0:c0 + na * 128], ALU.mult)
        if self.stop == ("cdsp", l):
            self.dbg_dump(self.AT)
            raise StopBuild()
        DG = self.DG
        q = 0
        CV = int(os.environ.get("CVCUT", "9"))
        for ti, (t0, sz) in enumerate(TILES):
            if ti == 0 and not ctx_out:
                continue
            if CV < 2:
                continue
            gc = gcol(t0)
            for c in range(4):
                ps = PS[2 + q % 2]
                q += 1
                for k in range(31):
                    K.mm(ps[:, :sz], DG[:, c, k, :], GLU[:, c, gc + k - 15:gc + k - 15 + sz], start=(k == 0), stop=(k == 30))
                cv = self.CV2[q % 2]
                dw, dsq = cv["dw"], cv["dsq"]
                K.act(dw[:, :sz], ps[:, :sz], AF.Identity, bias=self.CONVV[:, i, c, 0:1])
                if CV < 3:
                    continue
                K.act(dsq[:, :sz], ps[:, :sz], AF.Square, bias=self.CONVV[:, i, c, 0:1])
                s1, s2 = PS[4 + 2 * (q % 2)], PS[5 + 2 * (q % 2)]
                K.mm(s1[:, :sz], self.ONESG[:], dw[:, :sz])
                K.mm(s2[:, :sz], self.ONESG[:], dsq[:, :sz])
                M, VV = cv["M"], cv["VV"]
                K.copy(M[:, :sz], s1[:, :sz], eng="act")
                K.act(dsq[:, :sz], s1[:, :sz], AF.Square)
                K.tt(VV[:, :sz], s2[:, :sz], dsq[:, :sz], ALU.subtract)
                self.rstd(VV[:, :sz], VV[:, :sz], 1)
                if CV < 4:
                    continue
                K.tt(dw[:, :sz], dw[:, :sz], M[:, :sz], ALU.subtract)
                K.tt(dw[:, :sz], dw[:, :sz], VV[:, :sz], ALU.mult)
                K.ts(dw[:, :sz], dw[:, :sz], self.CONVV[:, i, c, 1:2], self.CONVV[:, i, c, 2:3], ALU.mult, ALU.add)
                if os.environ.get("NOSILU"):
                    K.copy(YT[:, 4 + c, t0:t0 + sz], dw[:, :sz])
                else:
                    self.sigmoid(dsq[:, :sz], dw[:, :sz])
                    K.tt(YT[:, 4 + c, t0:t0 + sz], dw[:, :sz], dsq[:, :sz], ALU.mult)

    def moe(self, s, l, ctx_out):
        K = self.K
        X, H, PS = self.X, self.H, self.PS
        RT, RL = self.RT, self.RL
        K.dma("sp", self.WR[:], self.w_route[l].rearrange("(c p) n -> p c n", p=128))
        tiles = [(ti, t0, sz) for ti, (t0, sz) in enumerate(TILES) if not (ti == 0 and not ctx_out)]
        ptb = PS[1][:, 0:64].bitcast(BF16)

        def route_chunk(tc):
                c0 = tc * 128
                n = 2 if tc < 2 else s
                for c in range(8):
                    K.act(self.HF[:, c, :], X[:, c, c0:c0 + 128], AF.Identity, bias=self.mod(l, 3, c, n), scale=self.mod(l, 4, c, n))
                pl = PS[0]
                for c in range(8):
                    K.mm(pl[:, 0:36], self.HF[:, c, :], self.WR[:, c, :], start=(c == 0), stop=(c == 7))
                K.tt(RL[:], pl[:, 0:36], self.BRT[:, l, :], ALU.add)
                K.strict = True
                gmax, ngmax, gsum, gw = RT[:, 0:1], RT[:, 1:2], RT[:, 2:3], RT[:, 3:4]
                OH, ge = RT[:, 4:8], RT[:, 8:12]
                d, e2, den, r = RT[:, 12:13], RT[:, 13:14], RT[:, 14:15], RT[:, 15:16]
                esel, mask1, mask2 = RT[:, 16:24], RT[:, 24:32], RT[:, 32:40]
                w1, w2, wsel = RT[:, 40:41], RT[:, 41:42], RT[:, 48:56]
                K.op("dve", lambda e: e.reduce_max(gmax, RL[:, 0:4], AX.X), r=(RL[:, 0:4],), w=(gmax,))
                K.ts(OH, RL[:, 0:4], gmax, None, ALU.is_equal)
                K.ts(ngmax, gmax, -1.0, None, ALU.mult)
                K.act(ge, RL[:, 0:4], AF.Exp, bias=ngmax, accum_out=gsum)
                K.recip(gw, gsum)
                K.ts(esel, RL[:, 4:12], OH[:, 0:1], None, ALU.mult)
                for g in range(1, 4):
                    K.stt(esel, RL[:, 4 + 8 * g:12 + 8 * g], OH[:, g:g + 1], esel, ALU.mult, ALU.add)
                top = self.RTOP[:, 0:8]
                K.op("dve", lambda e: e.max(top, esel), r=(esel,), w=(top,))
                m1, m2 = top[:, 0:1], top[:, 1:2]
                K.ts(mask1, esel, m1, None, ALU.is_equal)
                K.ts(mask2, esel, m2, None, ALU.is_equal)
                K.tt(d, m2, m1, ALU.subtract)
                K.act(e2, d, AF.Exp)
                K.ts(den, e2, 1.0, None, ALU.add)
                K.recip(r, den)
                K.tt(w1, r, gw, ALU.mult)
                K.tt(w2, w1, e2, ALU.mult)
                K.ts(wsel, mask1, w1, None, ALU.mult)
                K.stt(wsel, mask2, w2, wsel, ALU.mult, ALU.add)
                K.tt(self.RCMB[:].rearrange("p (g j) -> p g j", j=8), OH.unsqueeze(2).to_broadcast([128, 4, 8]),
                     wsel.unsqueeze(1).to_broadcast([128, 4, 8]), ALU.mult)
                K.copy(self.RCB2[:, 0:32], self.RCMB[:])
                K.tt(self.RCB2[:, 32:64], self.RCMB[:], self.RCB2[:, 0:32], ALU.subtract)
                K.strict = False
                K.transpose(ptb[0:64, 0:128], self.RCB2[:, 0:64], self.IDB[:])
                K.copy(self.COMBT[0:64, c0:c0 + 128], ptb[0:64, 0:128])

        jobs = []
        for ep in range(16):
            for (ti, t0, sz) in tiles:
                jobs.append((ep, ti, t0, sz))
        q = 0
        pq = 0

        def load_w(ep):
            slot = ep % 2
            WG, WU, WD = self.WG[slot], self.WU[slot], self.WD[slot]
            for e2_ in range(2):
                e = ep * 2 + e2_
                K.dma("pool", WG[:, e2_, :, :], self.w_exp_gate[l, e].rearrange("(c p) f -> p c f", p=128))
                K.dma("pool", WU[:, e2_, :, :], self.w_exp_up[l, e].rearrange("(c p) f -> p c f", p=128))
                K.dma("pool", WD[:, e2_, :, :], self.w_exp_down[l, e].rearrange("(c p) f -> p c f", p=128))

        def gu(j):
            nonlocal q
            ep, ti, t0, sz = jobs[j]
            slot = ep % 2
            WG, WU = self.WG[slot], self.WU[slot]
            hid = self.HID[j % 2]
            for e2_ in range(2):
                e = ep * 2 + e2_
                cb = PS[4 + e2_]
                K.mm(cb[:, :sz], self.SEL[0:64, e, :], self.COMBT[0:64, t0:t0 + sz])
                for fcn in range(2):
                    pg, pu = PS[(q % 2) * 2], PS[(q % 2) * 2 + 1]
                    sg, tm = self.SG[q % 2], self.TM[q % 2]
                    q += 1
                    for k in range(8):
                        K.mm(pg[:, :sz], WG[:, e2_, k, fcn * 128:(fcn + 1) * 128], H[:, k, t0:t0 + sz], start=(k == 0), stop=(k == 7))
                    for k in range(8):
                        K.mm(pu[:, :sz], WU[:, e2_, k, fcn * 128:(fcn + 1) * 128], H[:, k, t0:t0 + sz], start=(k == 0), stop=(k == 7))
                    K.act(sg[:, :sz], pg[:, :sz], AF.Silu)
                    K.tt(tm[:, :sz], pu[:, :sz], sg[:, :sz], ALU.mult)
                    K.tt(hid[:, e2_ * 2 + fcn, :sz], tm[:, :sz], cb[:, :sz], ALU.mult)

        def down(j):
            nonlocal pq
            ep, ti, t0, sz = jobs[j]
            n = 2 if ti == 0 else s
            WD = self.WD[ep % 2]
            hid = self.HID[j % 2]
            for oc in range(8):
                py = PS[6 + pq % 2]
                pq += 1
                idx = 0
                for e2_ in range(2):
                    for fcn in range(2):
                        K.mm(py[:, :sz], WD[:, e2_, fcn, oc * 128:(oc + 1) * 128], hid[:, e2_ * 2 + fcn, :sz], start=(idx == 0), stop=(idx == 3))
                        idx += 1
                K.stt(X[:, oc, t0:t0 + sz], py[:, :sz], self.mod(l, 5, oc, n), X[:, oc, t0:t0 + sz], ALU.mult, ALU.add)

        for j, (ep, ti, t0, sz) in enumerate(jobs):
            if ti == tiles[0][0]:
                load_w(ep)
            if ep == 0:
                for tc in range(t0 // 128, (t0 + sz) // 128):
                    route_chunk(tc)
            gu(j)
            if j >= 1:
                down(j - 1)
        down(len(jobs) - 1)
        for (ti, t0, sz) in tiles:
            self.ln_tile(l, 1, t0, sz, 0)


def kernel(**inputs):
    inp = {k: np.asarray(v) for k, v in inputs.items()}
    maps = host_prep(inp)
    p = Prog()
    res = run_bass_kernel_spmd(p.nc, maps, core_ids=list(range(8)))
    out = np.empty((16, NLAT, D), np.float32)
    for i in range(8):
        o = np.asarray(res.results[i]["outT"])
        for s in range(2):
            out[2 * i + s] = o[s].T
    return out
```
